# Optimizing a Trainium2 kernel written in Bass

```python
import jax, jax.numpy as jnp
from jax import lax
import numpy as np

D_MODEL = 1024
BATCH = 8
SEQ = 2048
DEPTH = 2

N_MIXERS = 2
N_HEADS = 4
QK_DIM = D_MODEL // 2 // N_HEADS
V_DIM = D_MODEL // N_HEADS
QK_W = N_HEADS * QK_DIM
V_W = N_HEADS * V_DIM
CHUNK = 64
D_FF = 4 * D_MODEL
GLA_GATE_RANK = 16
GLA_TAU = 16.0
EPS = 1e-6
MLSTM_IN = 2 * QK_W + 2 * V_W + 2 * N_HEADS
GLA_IN = 2 * QK_W + 2 * V_W + GLA_GATE_RANK
N_MLSTM_LAYERS = (DEPTH + 1) // 2
N_GLA_LAYERS = DEPTH // 2

kernel_name = 'hybrid_mlstm_gla_sqrelu'


def rmsnorm(x, g):
    xf = x.astype(jnp.float32)
    y = xf * lax.rsqrt(jnp.mean(xf * xf, axis=-1, keepdims=True) + EPS)
    return (y * g.astype(jnp.float32)).astype(x.dtype)


def head_rmsnorm(h, g):
    B, S, H, dv = h.shape
    y = h * lax.rsqrt(jnp.mean(h * h, axis=-1, keepdims=True) + EPS)
    return y.reshape(B, S, H * dv) * g.astype(jnp.float32)


def to_chunks(t):
    B, S, H, d = t.shape
    return t.reshape(B, S // CHUNK, CHUNK, H, d).transpose(1, 0, 3, 2, 4)


def gate_chunks(t):
    B, S, H = t.shape
    return t.reshape(B, S // CHUNK, CHUNK, H).transpose(1, 0, 3, 2)


def from_chunks(t):
    NC, B, H, L, d = t.shape
    return t.transpose(1, 0, 3, 2, 4).reshape(B, NC * L, H, d)


def mlstm_chunkwise(q, k, v, i_pre, f_pre):
    B, S, H, dk = q.shape
    dv = v.shape[-1]
    causal = jnp.tril(jnp.ones((CHUNK, CHUNK), dtype=bool))
    xs = (to_chunks(q), to_chunks(k), to_chunks(v), gate_chunks(i_pre), gate_chunks(jax.nn.log_sigmoid(f_pre)))

    def step(carry, inp):
        C, n, m = carry
        qj, ks, vs, li, lf = inp
        b = jnp.cumsum(lf, axis=-1)
        g = b[..., -1]
        dmat = jnp.where(causal, b[..., :, None] - b[..., None, :] + li[..., None, :], -jnp.inf)
        inter = b + m[..., None]
        m_row = jnp.maximum(inter, jnp.max(dmat, axis=-1))
        w_inter = jnp.exp(inter - m_row)
        s = jnp.einsum('bhjd,bhsd->bhjs', qj, ks) * jnp.exp(dmat - m_row[..., None])
        num = w_inter[..., None] * jnp.einsum('bhjd,bhde->bhje', qj, C) + jnp.einsum('bhjs,bhse->bhje', s, vs)
        den = w_inter * jnp.einsum('bhjd,bhd->bhj', qj, n) + jnp.sum(s, axis=-1)
        h = num / jnp.maximum(jnp.abs(den), jnp.exp(-m_row))[..., None]
        a = g[..., None] - b + li
        m_new = jnp.maximum(g + m, jnp.max(a, axis=-1))
        w_s = jnp.exp(a - m_new[..., None])
        decay = jnp.exp(g + m - m_new)
        C_new = decay[..., None, None] * C + jnp.einsum('bhs,bhsd,bhse->bhde', w_s, ks, vs)
        n_new = decay[..., None] * n + jnp.einsum('bhs,bhsd->bhd', w_s, ks)
        return (C_new, n_new, m_new), h

    init = (jnp.zeros((B, H, dk, dv), jnp.float32), jnp.zeros((B, H, dk), jnp.float32), jnp.zeros((B, H), jnp.float32))
    _, h = lax.scan(step, init, xs)
    return from_chunks(h)


def gla_chunkwise(q, k, v, log_a):
    B, S, H, dk = q.shape
    dv = v.shape[-1]
    causal = jnp.tril(jnp.ones((CHUNK, CHUNK), dtype=bool))[:, :, None]
    xs = (to_chunks(q), to_chunks(k), to_chunks(v), to_chunks(log_a))

    def step(S_state, inp):
        qj, ks, vs, la = inp
        b = jnp.cumsum(la, axis=-2)
        g = b[..., -1, :]
        rel = jnp.where(causal, b[..., :, None, :] - b[..., None, :, :], -jnp.inf)
        A = jnp.einsum('bhjd,bhsd,bhjsd->bhjs', qj, ks, jnp.exp(rel))
        o = jnp.einsum('bhjs,bhse->bhje', A, vs) + jnp.einsum('bhjd,bhde->bhje', qj * jnp.exp(b), S_state)
        S_new = jnp.exp(g)[..., None] * S_state + jnp.einsum('bhsd,bhse->bhde', ks * jnp.exp(g[..., None, :] - b), vs)
        return S_new, o

    _, o = lax.scan(step, jnp.zeros((B, H, dk, dv), jnp.float32), xs)
    return from_chunks(o)


def mlstm_mixer(xn, w_in, b_gate, out_norm_g, w_out):
    B, S, _ = xn.shape
    proj = xn @ w_in
    q, k, v, o_pre, gates = jnp.split(proj, [QK_W, 2 * QK_W, 2 * QK_W + V_W, 2 * QK_W + 2 * V_W], axis=-1)
    gates = gates.astype(jnp.float32) + b_gate.astype(jnp.float32)
    i_pre, f_pre = gates[..., :N_HEADS], gates[..., N_HEADS:]
    q = q.reshape(B, S, N_HEADS, QK_DIM).astype(jnp.float32)
    k = k.reshape(B, S, N_HEADS, QK_DIM).astype(jnp.float32) * (QK_DIM ** -0.5)
    v = v.reshape(B, S, N_HEADS, V_DIM).astype(jnp.float32)
    h_tilde = mlstm_chunkwise(q, k, v, i_pre, f_pre)
    h = jax.nn.sigmoid(o_pre.astype(jnp.float32)).reshape(B, S, N_HEADS, V_DIM) * h_tilde
    return head_rmsnorm(h, out_norm_g).astype(xn.dtype) @ w_out


def gla_mixer(xn, w_in, w_gate_up, b_gate, out_norm_g, w_out):
    B, S, _ = xn.shape
    proj = xn @ w_in
    q, k, v, r, z_low = jnp.split(proj, [QK_W, 2 * QK_W, 2 * QK_W + V_W, 2 * QK_W + 2 * V_W], axis=-1)
    z = (z_low @ w_gate_up).astype(jnp.float32) + b_gate.astype(jnp.float32)
    log_a = (jax.nn.log_sigmoid(z) / GLA_TAU).reshape(B, S, N_HEADS, QK_DIM)
    q = q.reshape(B, S, N_HEADS, QK_DIM).astype(jnp.float32) * (QK_DIM ** -0.5)
    k = k.reshape(B, S, N_HEADS, QK_DIM).astype(jnp.float32)
    v = v.reshape(B, S, N_HEADS, V_DIM).astype(jnp.float32)
    o = head_rmsnorm(gla_chunkwise(q, k, v, log_a), out_norm_g)
    return (jax.nn.silu(r.astype(jnp.float32)) * o).astype(xn.dtype) @ w_out


def sq_relu_mlp(xn, w1, w2):
    hid = jnp.square(jax.nn.relu(xn @ w1))
    return hid @ w2


def setup_inputs(seed: int = 0) -> dict:
    key = jax.random.key(seed)
    ks = jax.random.split(key, 20)
    f32 = jnp.float32

    def nrm(k, shape, fan_in):
        return jax.random.normal(k, shape, f32) * (fan_in ** -0.5)

    na, nb = N_MLSTM_LAYERS, N_GLA_LAYERS
    x = jax.random.normal(ks[0], (BATCH, SEQ, D_MODEL), f32)
    norm_mix_g = 1.0 + 0.02 * jax.random.normal(ks[1], (DEPTH, D_MODEL), f32)
    norm_ffn_g = 1.0 + 0.02 * jax.random.normal(ks[2], (DEPTH, D_MODEL), f32)
    final_norm_g = 1.0 + 0.02 * jax.random.normal(ks[3], (D_MODEL,), f32)
    mlstm_w_in = nrm(ks[4], (na, D_MODEL, MLSTM_IN), D_MODEL)
    i_bias = 0.1 * jax.random.normal(ks[5], (na, N_HEADS), f32)
    f_bias = 3.0 + 3.0 * jax.random.uniform(ks[6], (na, N_HEADS), f32)
    mlstm_b_gate = jnp.concatenate([i_bias, f_bias], axis=-1)
    mlstm_out_norm_g = 1.0 + 0.02 * jax.random.normal(ks[7], (na, V_W), f32)
    mlstm_w_out = nrm(ks[8], (na, V_W, D_MODEL), V_W)
    gla_w_in = nrm(ks[9], (nb, D_MODEL, GLA_IN), D_MODEL)
    gla_w_gate_up = nrm(ks[10], (nb, GLA_GATE_RANK, QK_W), GLA_GATE_RANK)
    gla_b_gate = 0.1 * jax.random.normal(ks[11], (nb, QK_W), f32)
    gla_out_norm_g = 1.0 + 0.02 * jax.random.normal(ks[12], (nb, V_W), f32)
    gla_w_out = nrm(ks[13], (nb, V_W, D_MODEL), V_W)
    ffn_w1 = nrm(ks[14], (DEPTH, D_MODEL, D_FF), D_MODEL)
    ffn_w2 = nrm(ks[15], (DEPTH, D_FF, D_MODEL), D_FF)
    return {'x': x, 'norm_mix_g': norm_mix_g, 'norm_ffn_g': norm_ffn_g, 'final_norm_g': final_norm_g,
            'mlstm_w_in': mlstm_w_in, 'mlstm_b_gate': mlstm_b_gate, 'mlstm_out_norm_g': mlstm_out_norm_g,
            'mlstm_w_out': mlstm_w_out, 'gla_w_in': gla_w_in, 'gla_w_gate_up': gla_w_gate_up,
            'gla_b_gate': gla_b_gate, 'gla_out_norm_g': gla_out_norm_g, 'gla_w_out': gla_w_out,
            'ffn_w1': ffn_w1, 'ffn_w2': ffn_w2}


def reference(x, norm_mix_g, norm_ffn_g, final_norm_g, mlstm_w_in, mlstm_b_gate, mlstm_out_norm_g,
              mlstm_w_out, gla_w_in, gla_w_gate_up, gla_b_gate, gla_out_norm_g, gla_w_out, ffn_w1, ffn_w2):
    for i in range(DEPTH):
        j = i // N_MIXERS
        h = rmsnorm(x, norm_mix_g[i])
        if i % N_MIXERS == 0:
            x = x + mlstm_mixer(h, mlstm_w_in[j], mlstm_b_gate[j], mlstm_out_norm_g[j], mlstm_w_out[j])
        else:
            x = x + gla_mixer(h, gla_w_in[j], gla_w_gate_up[j], gla_b_gate[j], gla_out_norm_g[j], gla_w_out[j])
        h = rmsnorm(x, norm_ffn_g[i])
        x = x + sq_relu_mlp(h, ffn_w1[i], ffn_w2[i])
    return rmsnorm(x, final_norm_g)
```

```python
import numpy as np
from contextlib import ExitStack
import concourse.bass as bass
import concourse.mybir as mybir
from concourse.bass_utils import run_bass_kernel_spmd

F32 = mybir.dt.float32
BF16 = mybir.dt.bfloat16
AF = mybir.ActivationFunctionType
ALU = mybir.AluOpType

T = 2048
D = 1024
KC = 8
NG = 4
GS = 512
CH = 128
H = 4
DK = 128
DV = 256
DFF = 4096
EPS = 1e-6
QSCALE = DK ** -0.5


class P:
    def __init__(s, nc, es):
        s.nc = nc
        s.es = es
        s.E = {}
        for name, eng in (("pe", nc.tensor), ("act", nc.scalar), ("dve", nc.vector),
                          ("pool", nc.gpsimd), ("sp", nc.sync)):
            s.E[name] = dict(eng=eng, sem=es.enter_context(nc.semaphore("s_" + name)), count=0, waited={})
        s.lastw = {}
        s.readers = {}
        s.dsem = {}
        s.uid = 0

    def name(s, base):
        s.uid += 1
        return "%s_%d" % (base, s.uid)

    def _waits(s, ename, reads, writes):
        e = s.E[ename]
        pesem = s.E["pe"]["sem"]
        need = {}

        def add(sem, val):
            if ename == "pe" and sem is pesem:
                return
            if need.get(sem, 0) < val:
                need[sem] = val
        for k in reads:
            t = s.lastw.get(k)
            if t:
                add(*t)
        for k in writes:
            t = s.lastw.get(k)
            if t:
                add(*t)
            for sem, val in s.readers.get(k, {}).items():
                add(sem, val)
        for sem, val in need.items():
            if e["waited"].get(sem, 0) < val:
                e["eng"].wait_ge(sem, val)
                e["waited"][sem] = val

    def _reg(s, tok, reads, writes):
        for k in reads:
            d = s.readers.setdefault(k, {})
            if d.get(tok[0], 0) < tok[1]:
                d[tok[0]] = tok[1]
        for k in writes:
            s.lastw[k] = tok
            s.readers[k] = {}

    @staticmethod
    def _split(reads, writes):
        def nk(k):
            if k[0] in ("sc", "num", "dC"):
                return (k[0],), True
            if k[0] in ("tp", "small"):
                return ("misc",), True
            if k[0] == "acc":
                return k, True
            return k, False
        r2, w2 = [], []
        for k in reads:
            k2, ex = nk(k)
            (w2 if ex else r2).append(k2)
        for k in writes:
            w2.append(nk(k)[0])
        return r2, w2

    def op(s, ename, fn, reads=(), writes=(), signal=True):
        reads, writes = s._split(reads, writes)
        s._waits(ename, reads, writes)
        e = s.E[ename]
        ins = fn(e["eng"])
        if signal:
            e["count"] += 1
            ins.then_inc(e["sem"], 1)
            tok = (e["sem"], e["count"])
        else:
            tok = (e["sem"], e["count"] + 1)
        s._reg(tok, reads, writes)

    def dma(s, qname, dname, out, in_=None, reads=(), writes=()):
        pairs = out if in_ is None else [(out, in_)]
        s._waits(qname, reads, writes)
        if dname not in s.dsem:
            s.dsem[dname] = [s.es.enter_context(s.nc.semaphore("d_" + dname)), 0]
        d = s.dsem[dname]
        for o, i in pairs:
            d[1] += 16
            s.E[qname]["eng"].dma_start(out=o, in_=i).then_inc(d[0], 16)
        s._reg((d[0], d[1]), reads, writes)
        return (d[0], d[1])

    def barrier(s):
        toks = [(e["sem"], e["count"]) for e in s.E.values() if e["count"] > 0]
        toks += [(d[0], d[1]) for d in s.dsem.values()]
        pesem = s.E["pe"]["sem"]
        for name, e in s.E.items():
            for sem, val in toks:
                if name == "pe" and sem is pesem:
                    continue
                if e["waited"].get(sem, 0) < val:
                    e["eng"].wait_ge(sem, val)
                    e["waited"][sem] = val


def build(dbg=None):
    nc = bass.Bass("TRN2", target_bir_lowering=False)

    def dram(name, shape, kind="ExternalInput"):
        return nc.dram_tensor(name, list(shape), F32, kind=kind).ap()

    xT_d = dram("xT", [D, T])
    gains_d = dram("gains", [128, 40])
    m_win_d = dram("m_win", [D, 3080])
    m_bg_d = dram("m_bg", [1, 8])
    m_ong_d = dram("m_ong", [1, 1024])
    m_ongp_d = dram("m_ongp", [128, 8])
    m_wout_d = dram("m_wout", [D, D])
    g_win_d = dram("g_win", [D, 3088])
    g_wgu_d = dram("g_wgu", [16, 512])
    g_bg_d = dram("g_bg", [128, 4])
    g_ong_d = dram("g_ong", [1, 1024])
    g_wout_d = dram("g_wout", [D, D])
    w1_d = dram("w1", [2, D, DFF])
    w2_d = dram("w2", [2, DFF, D])
    out_d = dram("outT", [D, T], kind="ExternalOutput")

    es = ExitStack()
    p = P(nc, es)

    def sb(stack, base, shape, dt):
        return stack.enter_context(nc.sbuf_tensor(p.name(base), list(shape), dt))

    def ps(base, shape, dt):
        return es.enter_context(nc.psum_tensor(p.name(base), list(shape), dt))

    xT = sb(es, "xT", [128, KC, T], F32)
    gains = sb(es, "gains", [128, 40], F32)
    ident = sb(es, "ident", [128, 128], BF16)
    tri_f = sb(es, "tri_f", [128, 128], F32)
    ones_f = sb(es, "ones_f", [128, 128], F32)
    ones_bf = sb(es, "ones_bf", [128, 128], BF16)
    rmask = sb(es, "rmask", [128, GS], F32)

    acc = [ps("acc", [128, 512], F32) for _ in range(4)]
    sc = ps("sc", [128, 4, 128], F32)
    num = ps("num", [128, 2, 256], F32)
    dC = ps("dC", [128, 2, 256], F32)
    tpsm = ps("tpsm", [128, 512], F32)
    tp = tpsm[:, 0:256].bitcast(BF16).rearrange("p (s n) -> p s n", n=128)
    small = tpsm[:, 256:512]
    rr = dict(accm=0, acc=0, sc=0, num=0, dC=0, tp=0, den=0, rt=0)

    def nxt(nm, n):
        i = rr[nm] % n
        rr[nm] = (i + 1) % n
        return i

    def emit_consts():
        p.op("dve", lambda e: e.memset(ident[:], 0.0), writes=[("ident",)])
        p.op("dve", lambda e: e.memset(ones_f[:], 1.0), writes=[("ones_f",)])
        p.op("dve", lambda e: e.memset(ones_bf[:], 1.0), writes=[("ones_bf",)])
        p.op("dve", lambda e: e.memset(tri_f[:], 1.0), writes=[("tri_f",)])
        p.op("dve", lambda e: e.memset(rmask[:], 1.0), writes=[("rmask",)])
        p.op("dve", lambda e: e.memset(rmask[:].rearrange("p (c t) -> p c t", t=CH)[:, :, 0:1], 0.0),
             reads=[("rmask",)], writes=[("rmask",)])
        p.op("pool", lambda e: e.affine_select(out=ident[:], in_=ident[:], compare_op=ALU.not_equal, fill=1.0,
                                               base=0, pattern=[[-1, 128]], channel_multiplier=1),
             reads=[("ident",)], writes=[("ident",)])
        p.op("pool", lambda e: e.affine_select(out=tri_f[:], in_=tri_f[:], compare_op=ALU.is_ge, fill=0.0,
                                               base=0, pattern=[[1, 128]], channel_multiplier=-1),
             reads=[("tri_f",)], writes=[("tri_f",)])

    p.dma("sp", "gains", gains[:], gains_d, writes=[("gains",)])
    xT_v = xT_d.rearrange("(k p) t -> p k t", p=128)
    def load_xT(q, tg):
        p.dma(q, "xT%d" % tg, [(xT[:, k0:k0 + 4, tg * GS:(tg + 1) * GS], xT_v[:, k0:k0 + 4, tg * GS:(tg + 1) * GS])
                               for k0 in (0, 4)],
              writes=[("xT", k, tg) for k in range(KC)])
    load_xT("sp", 0)

    def tgs(tg):
        return slice(tg * GS, (tg + 1) * GS)

    def norm_stats(tg, sq, sd, rstd, banks=None):
        a = nxt("acc", 4) if banks is None else banks[nxt("accm", len(banks))]
        for k in range(KC):
            sl = k % 2
            p.op("act", lambda e, k=k, sl=sl: e.activation(out=sq[:, sl, :], in_=xT[:, k, tgs(tg)], func=AF.Square),
                 reads=[("xT", k, tg)], writes=[("sq", sl)])
            p.op("pe", lambda e, k=k, sl=sl: e.matmul(acc[a][:], lhsT=ones_bf[:], rhs=sq[:, sl, :],
                                                     start=(k == 0), stop=(k == KC - 1)),
                 reads=[("sq", sl), ("ones_bf",)], writes=[("acc", a)], signal=True)
        p.op("act", lambda e: e.activation(out=sd[:], in_=acc[a][:], func=AF.Sqrt, scale=1.0 / D, bias=EPS),
             reads=[("acc", a)], writes=[("sd",)])
        p.op("dve", lambda e: e.reciprocal(out=rstd[:], in_=sd[:]), reads=[("sd",)], writes=[("rstd",)])

    def dump_and_finish():
        p.dma("sp", "out", [(out_d[k * 128:(k + 1) * 128, :], xT[:, k, :]) for k in range(KC)],
              reads=[("xT", k, tg) for k in range(KC) for tg in range(NG)])
        d = p.dsem["out"]
        nc.sync.wait_ge(d[0], d[1])
        es.close()
        return nc

    def mixer(layer, kind):
        is_m = (kind == "mlstm")
        win_d = m_win_d if is_m else g_win_d
        wout_d = m_wout_d if is_m else g_wout_d
        ong_d = m_ong_d if is_m else g_ong_d
        NIN = 3080 if is_m else 3088
        gcol = layer * 8
        numP = [num, acc[2][:].rearrange("p (s n) -> p s n", n=DV)]
        numK = [("num",), ("acc", 2)]
        dCP = [dC, dC]
        dCK = [("dC",), ("dC",)]
        PB = [0, 1, 3]
        tpS = sc[:].rearrange("p s n -> p (s n)").bitcast(BF16).rearrange("p (s n) -> p s n", n=128)
        tpK = dC[:].rearrange("p s n -> p (s n)").bitcast(BF16).rearrange("p (s n) -> p s n", n=128)
        with ExitStack() as ph:
            win = sb(ph, "win", [128, KC, NIN], BF16)
            wout = sb(ph, "wout", [128, KC, D], BF16)
            ong_b = sb(ph, "ong_b", [128, 1024], F32)
            xn = sb(ph, "xn", [128, KC, GS], BF16)
            sq = sb(ph, "sq", [128, 2, GS], BF16)
            sd = sb(ph, "sd", [128, GS], F32)
            rstd = sb(ph, "rstd", [128, GS], F32)
            qT = sb(ph, "qT", [128, H, GS], BF16)
            kT = sb(ph, "kT", [128, H, GS], BF16)
            ktok = [sb(ph, "ktok", [128, H, DK], BF16) for _ in range(2)]
            vt = [sb(ph, "vt", [128, H, DV], BF16) for _ in range(2)]
            og = [sb(ph, "og", [128, H * DV], F32) for _ in range(2)]
            St = sb(ph, "St", [128, H, 128], BF16)
            tri4 = sb(ph, "tri4", [128, H, 128], BF16)
            junk = sb(ph, "junk", [128, DV], BF16)
            hn = sb(ph, "hn", [128, H, DV], BF16)
            hnT = [sb(ph, "hnT", [128, KC, GS], BF16) for _ in range(2)]
            R32 = sb(ph, "R32", [128, H, DV], F32)
            Cbf = sb(ph, "Cbf", [128, H, DV], BF16)
            sm = sb(ph, "sm", [128, 16, 16], F32)
            egs = sb(ph, "egs", [128, 2, 16], F32)
            tiny = sb(ph, "tiny", [128, 16, 4], F32)
            if is_m:
                hg = sb(ph, "hg", [128, H, DV], F32)
                n32 = sb(ph, "n32", [128, H], F32)
                nbf = sb(ph, "nbf", [128, H], BF16)
                ubf = sb(ph, "ubf", [128, 2, 16], BF16)
                bg4 = sb(ph, "bg4", [128, 4, 8], F32)
                ongp = sb(ph, "ongp", [128, 8], F32)
                gates = sb(ph, "gates", [128, 4, 8], F32)
            else:
                wgu = sb(ph, "wgu", [16, 512], F32)
                gbg = sb(ph, "gbg", [128, 4], F32)
                ngbg = sb(ph, "ngbg", [128, 4], F32)
                zl = sq[0:16, :, :].rearrange("p a t -> p (a t)").bitcast(F32)
                e1 = sd[:].rearrange("p (o t) -> p o t", o=1)
                nbT = rstd[:].rearrange("p (o t) -> p o t", o=1)
                EB = sb(ph, "EB", [128, 1, GS], F32)
                EK = sb(ph, "EK", [128, 1, GS], F32)

            if dbg == 'mem':
                print('SBUF free in mixer phase', kind, nc.sbuf_bytes_remaining)
            win_src = win_d.rearrange("(k p) n -> p k n", p=128)
            p.dma("pool", "winZ", [(win[:, :, 3072:NIN], win_src[:, :, 3072:NIN])], writes=[("win", "gate")])
            p.dma("pool", "winA", [(win[:, k0:k0 + 4, 0:1024], win_src[:, k0:k0 + 4, 0:1024]) for k0 in range(0, KC, 4)],
                  writes=[("win", "qk")])
            p.dma("pool", "winB", [(win[:, k0:k0 + 2, 1024:3072], win_src[:, k0:k0 + 2, 1024:3072]) for k0 in range(0, KC, 2)],
                  writes=[("win", "rest")])
            if layer == 0:
                for tg in range(1, NG):
                    load_xT("pool", tg)
                emit_consts()
            wout_src = wout_d.rearrange("(k p) n -> p k n", p=128)
            p.dma("pool", "wout", [(wout[:, k0:k0 + 4, :], wout_src[:, k0:k0 + 4, :]) for k0 in range(0, KC, 4)],
                  writes=[("wout",)])
            if is_m:
                p.dma("sp", "misc", [(ong_b[:], ong_d.partition_broadcast(128)), (ongp[:], m_ongp_d)] +
                      [(bg4[:, c, :], m_bg_d.partition_broadcast(128)) for c in range(4)],
                      writes=[("ong_b",), ("bg4",), ("ongp",)])
                for ec in range(KC):
                    p.op("act", lambda e, ec=ec: e.activation(out=wout[:, ec, :], in_=wout[:, ec, :], func=AF.Identity,
                                                              scale=ongp[:, ec:ec + 1]),
                         reads=[("wout",), ("ongp",)], writes=[("wout",)])
            else:
                p.dma("sp", "misc", [(ong_b[:], ong_d.partition_broadcast(128)), (wgu[:], g_wgu_d), (gbg[:], g_bg_d)],
                      writes=[("ong_b",), ("wgu",), ("gbg",)])
                p.op("dve", lambda e: e.tensor_scalar(out=ngbg[:], in0=gbg[:], scalar1=-1.0, scalar2=None,
                                                      op0=ALU.mult), reads=[("gbg",)], writes=[("ngbg",)])
            for h in range(H):
                p.op("pool", lambda e, h=h: e.tensor_copy(out=tri4[:, h, :], in_=tri_f[:]), reads=[("tri_f",)],
                     writes=[("tri4",)])

            state = dict(first=True)

            def proj_fm(col0, M, a):
                for k in range(KC):
                    p.op("pe", lambda e, k=k: e.matmul(acc[a][0:M, :], lhsT=win[:, k, col0:col0 + M],
                                                      rhs=xn[:, k, :], start=(k == 0), stop=(k == KC - 1)),
                         reads=[("win", "qk" if col0 < 1024 else ("gate" if col0 >= 3072 else "rest")), ("xn", k)], writes=[("acc", a)], signal=(k == KC - 1))

            def proj_tm(c, col0, N, a):
                for k in range(KC):
                    p.op("pe", lambda e, k=k: e.matmul(acc[a][:, 0:N], lhsT=xn[:, k, c * CH:(c + 1) * CH],
                                                      rhs=win[:, k, col0:col0 + N],
                                                      start=(k == 0), stop=(k == KC - 1)),
                         reads=[("win", "rest"), ("xn", k)], writes=[("acc", a)], signal=(k == KC - 1))

            c4 = lambda ap: ap.rearrange("p (c h) -> p c h", h=4)

            def stats_steps(g):
                return [lambda: norm_stats(g, sq, sd, rstd, banks=PB)]

            def preamble_steps(g):
                gp = g % 2
                steps = []

                def s_xn():
                    for k in range(KC):
                        p.op("dve", lambda e, k=k: e.scalar_tensor_tensor(
                            out=xn[:, k, :], in0=xT[:, k, tgs(g)], scalar=gains[:, gcol + k:gcol + k + 1],
                            in1=rstd[:], op0=ALU.mult, op1=ALU.mult),
                            reads=[("xT", k, g), ("gains",), ("rstd",)], writes=[("xn", k)])
                steps.append(s_xn)

                def s_q(h):
                    a = PB[nxt("accm", 3)]
                    proj_fm(h * DK, DK, a)
                    if is_m:
                        p.op("act", lambda e: e.activation(out=qT[:, h, :], in_=acc[a][:], func=AF.Copy),
                             reads=[("acc", a)], writes=[("qT", h)])
                    else:
                        p.op("dve", lambda e: e.scalar_tensor_tensor(
                            out=qT[:, h, :], in0=acc[a][:], scalar=QSCALE, in1=EB[:, 0, :], op0=ALU.mult, op1=ALU.mult),
                            reads=[("acc", a), ("EB",)], writes=[("qT", h)])

                def s_k(h):
                    a = PB[nxt("accm", 3)]
                    proj_fm(512 + h * DK, DK, a)
                    if is_m:
                        p.op("act", lambda e: e.activation(out=kT[:, h, :], in_=acc[a][:], func=AF.Copy, scale=QSCALE),
                             reads=[("acc", a)], writes=[("kT", h)])
                    else:
                        p.op("dve", lambda e: e.tensor_tensor(
                            out=kT[:, h, :], in0=acc[a][:], in1=EK[:, 0, :], op=ALU.mult),
                            reads=[("acc", a), ("EK",)], writes=[("kT", h)])

                if is_m:
                    ef, spt, tmpu = sm[:, 0, :], sm[:, 1, :], sm[:, 2, :]
                    u, eb = sm[:, 8 + gp, :], sm[:, 10 + gp, :]

                    def s_gates_a():
                        for c in range(4):
                            for k in range(KC):
                                p.op("pe", lambda e, k=k, c=c: e.matmul(
                                    small[:, c * 8:(c + 1) * 8], lhsT=xn[:, k, c * CH:(c + 1) * CH],
                                    rhs=win[:, k, 3072:3080], start=(k == 0), stop=(k == KC - 1)),
                                    reads=[("win", "gate"), ("xn", k)], writes=[("small", "gates")], signal=(k == KC - 1))
                        p.op("dve", lambda e: e.tensor_tensor(
                            out=gates[:], in0=small[:, 0:32].rearrange("p (c n) -> p c n", n=8), in1=bg4[:], op=ALU.add),
                            reads=[("small", "gates"), ("bg4",)], writes=[("gates",)])
                        p.op("act", lambda e: e.activation(out=c4(ef), in_=gates[:, :, 4:8], func=AF.Exp, scale=-1.0),
                             reads=[("gates",)], writes=[("sm", 0)])
                        p.op("act", lambda e: e.activation(out=spt, in_=ef, func=AF.Ln, bias=1.0),
                             reads=[("sm", 0)], writes=[("sm", 1)])

                    def s_gates_b():
                        p.op("pe", lambda e: e.matmul(small[:, 32:48], lhsT=tri_f[:], rhs=spt, start=True, stop=True),
                             reads=[("tri_f",), ("sm", 1)], writes=[("small", "bs")])
                        p.op("pe", lambda e: e.matmul(small[:, 48:64], lhsT=ones_f[:], rhs=spt, start=True, stop=True),
                             reads=[("ones_f",), ("sm", 1)], writes=[("small", "gs")])
                        p.op("dve", lambda e: e.tensor_tensor(out=c4(tmpu), in0=c4(small[:, 32:48]), in1=gates[:, :, 0:4],
                                                              op=ALU.add),
                             reads=[("small", "bs"), ("gates",)], writes=[("sm", 2)])
                        p.op("act", lambda e: e.activation(out=u, in_=tmpu, func=AF.Exp), reads=[("sm", 2)],
                             writes=[("sm", 8 + gp)])
                        p.op("act", lambda e: e.activation(out=eb, in_=small[:, 32:48], func=AF.Exp),
                             reads=[("small", "bs")], writes=[("sm", 10 + gp)])
                        p.op("act", lambda e: e.activation(out=egs[:, gp, :], in_=small[:, 48:64], func=AF.Exp, scale=-1.0),
                             reads=[("small", "gs")], writes=[("egs", gp)])
                        p.op("dve", lambda e: e.tensor_copy(out=ubf[:, gp, :], in_=u), reads=[("sm", 8 + gp)],
                             writes=[("ubf", gp)])
                    steps += [s_gates_a, lambda: s_q(0), lambda: s_k(0), s_gates_b]
                    for h in range(1, H):
                        steps += [lambda h=h: s_q(h), lambda h=h: s_k(h)]
                else:
                    def s_zl():
                        a = PB[nxt("accm", 3)]
                        proj_fm(3072, 16, a)
                        p.op("dve", lambda e: e.tensor_copy(out=zl, in_=acc[a][0:16, :]),
                             reads=[("acc", a)], writes=[("sq", 0), ("sq", 1)])

                    def s_z1(h):
                        a = PB[nxt("accm", 3)]
                        p.op("pe", lambda e: e.matmul(acc[a][:], lhsT=wgu[:, h * DK:(h + 1) * DK], rhs=zl,
                                                      start=True, stop=True),
                             reads=[("wgu",), ("sq", 0), ("sq", 1)], writes=[("acc", a)])
                        p.op("act", lambda e: e.activation(out=e1[:, 0, :], in_=acc[a][:], func=AF.Exp,
                                                           scale=-1.0, bias=ngbg[:, h:h + 1]),
                             reads=[("acc", a), ("ngbg",)], writes=[("sd",)])
                        p.op("act", lambda e: e.activation(out=e1[:, 0, :], in_=e1[:, 0, :], func=AF.Ln, bias=1.0),
                             reads=[("sd",)], writes=[("sd",)])
                        p.op("dve", lambda e: e.tensor_tensor_scan(out=nbT[:, 0, :], data0=rmask[:], data1=e1[:, 0, :],
                                                                  initial=0.0, op0=ALU.mult, op1=ALU.add),
                             reads=[("rmask",), ("sd",)], writes=[("rstd",)])

                    def s_z2(h):
                        p.op("act", lambda e: e.activation(out=EB[:, 0, :], in_=nbT[:, 0, :], func=AF.Exp,
                                                           scale=-1.0 / 16.0),
                             reads=[("rstd",)], writes=[("EB",)])
                        p.op("act", lambda e: e.activation(out=EK[:, 0, :], in_=nbT[:, 0, :], func=AF.Exp, scale=1.0 / 16.0),
                             reads=[("rstd",)], writes=[("EK",)])
                        p.op("dve", lambda e: e.tensor_copy(
                            out=egs[:, gp, h * 4:(h + 1) * 4],
                            in_=EB[:, 0, :].rearrange("p (c t) -> p c t", t=CH)[:, :, CH - 1]),
                            reads=[("EB",)], writes=[("egs", gp)])

                    def s_head(h):
                        a1 = PB[nxt("accm", 3)]
                        proj_fm(h * DK, DK, a1)
                        a2 = PB[nxt("accm", 3)]
                        proj_fm(512 + h * DK, DK, a2)
                        p.op("dve", lambda e: e.scalar_tensor_tensor(
                            out=qT[:, h, :], in0=acc[a1][:], scalar=QSCALE, in1=EB[:, 0, :], op0=ALU.mult, op1=ALU.mult),
                            reads=[("acc", a1), ("EB",)], writes=[("qT", h)])
                        p.op("dve", lambda e: e.tensor_tensor(
                            out=kT[:, h, :], in0=acc[a2][:], in1=EK[:, 0, :], op=ALU.mult),
                            reads=[("acc", a2), ("EK",)], writes=[("kT", h)])
                        if h + 1 < H:
                            s_z1(h + 1)
                            s_z2(h + 1)
                    steps += [s_zl, lambda: (s_z1(0), s_z2(0))]
                    for h in range(H):
                        steps.append(lambda h=h: s_head(h))
                return steps

            def proj_steps(g, c):
                par = c % 2
                gp = g % 2
                steps = []
                for vb in range(2):
                    def s_v(vb=vb):
                        a = PB[nxt("accm", 3)]
                        proj_tm(c, 1024 + vb * 512, 512, a)
                        if is_m:
                            for hh in range(2):
                                h = vb * 2 + hh
                                p.op("act", lambda e, h=h, hh=hh: e.activation(
                                    out=vt[par][:, h, :], in_=acc[a][:, hh * DV:(hh + 1) * DV], func=AF.Identity,
                                    scale=sm[:, 8 + gp, c * 4 + h:c * 4 + h + 1]),
                                    reads=[("acc", a), ("sm", 8 + gp)], writes=[("vt", par, vb)])
                        else:
                            p.op("act", lambda e: e.activation(
                                out=vt[par][:, vb * 2:vb * 2 + 2, :].rearrange("p h d -> p (h d)"), in_=acc[a][:],
                                func=AF.Copy),
                                reads=[("acc", a)], writes=[("vt", par, vb)])
                    steps.append(s_v)
                for vb in range(2):
                    def s_o(vb=vb):
                        a = PB[nxt("accm", 3)]
                        proj_tm(c, 2048 + vb * 512, 512, a)
                        if is_m:
                            p.op("act", lambda e: e.activation(out=og[par][:, vb * 512:(vb + 1) * 512], in_=acc[a][:],
                                                               func=AF.Sigmoid),
                                 reads=[("acc", a)], writes=[("og", par, vb)])
                        else:
                            p.op("act", lambda e: e.activation(out=og[par][:, vb * 512:(vb + 1) * 512], in_=acc[a][:],
                                                               func=AF.Silu),
                                 reads=[("acc", a)], writes=[("og", par, vb)])
                            p.op("pool", lambda e: e.tensor_tensor(
                                out=og[par][:, vb * 512:(vb + 1) * 512], in0=og[par][:, vb * 512:(vb + 1) * 512],
                                in1=ong_b[:, vb * 512:(vb + 1) * 512], op=ALU.mult),
                                reads=[("og", par, vb), ("ong_b",)], writes=[("og", par, vb)])
                    steps.append(s_o)
                return steps

            def outproj_steps(g):
                gp = g % 2
                steps = []
                for j in range(KC):
                    def s_j(j=j):
                        a = PB[nxt("accm", 3)]
                        for ec in range(KC):
                            p.op("pe", lambda e, ec=ec: e.matmul(
                                acc[a][:], lhsT=wout[:, ec, j * 128:(j + 1) * 128], rhs=hnT[gp][:, ec, :],
                                start=(ec == 0), stop=(ec == KC - 1)),
                                reads=[("wout",)] + [("hnT", gp, ec // 4, c) for c in range(4)], writes=[("acc", a)],
                                signal=(ec == KC - 1))
                        p.op("dve", lambda e: e.tensor_tensor(out=xT[:, j, tgs(g)], in0=acc[a][:], in1=xT[:, j, tgs(g)],
                                                              op=ALU.add),
                             reads=[("acc", a), ("xT", j, g)], writes=[("xT", j, g)])
                    steps.append(s_j)
                return steps

            def core_chunk(g, c, pend, after_s3):
                gp = g % 2
                cs = slice(c * CH, (c + 1) * CH)
                par = c % 2
                first = state["first"]

                def inter(n=1):
                    for _ in range(n):
                        if pend:
                            pend.pop(0)()

                if is_m:
                    egprev = (egs[:, gp, (c - 1) * 4:c * 4] if c > 0 else egs[:, 1 - gp, 12:16])
                    egcur = egs[:, gp, c * 4:(c + 1) * 4]
                else:
                    ev = lambda par_: egs[:, par_, :].rearrange("p (h c) -> p h c", c=4)
                    egprev = (ev(gp)[:, :, c - 1] if c > 0 else ev(1 - gp)[:, :, 3])
                    egcur = ev(gp)[:, :, c]
                egprev_key = ("egs", gp) if c > 0 else ("egs", 1 - gp)
                egcur_key = ("egs", gp)
                for h in range(H):
                    p.op("pe", lambda e, h=h: e.matmul(sc[:, h, :], lhsT=kT[:, h, cs], rhs=qT[:, h, cs],
                                                       start=True, stop=True),
                         reads=[("kT", h), ("qT", h)], writes=[("sc",)], signal=(h == H - 1))
                if True:
                    for h in range(H):
                        p.op("pe", lambda e, h=h: e.transpose(tpK[:, h, :], in_=kT[:, h, cs], identity=ident[:]),
                             reads=[("kT", h), ("ident",)], writes=[("dC",)], signal=(h == H - 1))
                    p.op("act", lambda e: e.activation(out=ktok[par][:], in_=tpK[:, 0:4, :], func=AF.Copy),
                         reads=[("dC",)], writes=[("ktok", par)])
                p.op("dve", lambda e: e.tensor_tensor(out=St[:], in0=sc[:, :, :], in1=tri4[:], op=ALU.mult),
                     reads=[("sc",), ("tri4",)], writes=[("St",)])
                inter(len(pend))
                for h in range(H):
                    pr, hs = h // 2, h % 2
                    p.op("pe", lambda e, h=h, pr=pr, hs=hs: e.matmul(numP[pr][:, hs, :], lhsT=St[:, h, :], rhs=vt[par][:, h, :],
                                                                    start=True, stop=first),
                         reads=[("St",), ("vt", par, h // 2)], writes=[numK[pr]], signal=(first and hs == 1))
                    if not first:
                        p.op("pe", lambda e, h=h, pr=pr, hs=hs: e.matmul(numP[pr][:, hs, :], lhsT=qT[:, h, cs], rhs=Cbf[:, h, :],
                                                                        start=False, stop=True),
                             reads=[("qT", h), ("Cbf",)], writes=[numK[pr]], signal=(hs == 1))
                if is_m:
                    for h in range(H):
                        p.op("pe", lambda e, h=h: e.matmul(
                            small[:, 64 + h:65 + h], lhsT=St[:, h, :], rhs=ubf[:, gp, c * 4 + h:c * 4 + h + 1],
                            start=True, stop=first),
                            reads=[("St",), ("ubf", gp)], writes=[("small", "den")], signal=False)
                        if not first:
                            p.op("pe", lambda e, h=h: e.matmul(
                                small[:, 64 + h:65 + h], lhsT=qT[:, h, cs], rhs=nbf[:, h:h + 1],
                                start=False, stop=True),
                                reads=[("qT", h), ("nbf",)], writes=[("small", "den")], signal=False)
                    for h in range(H):
                        p.op("pe", lambda e, h=h: e.matmul(
                            small[:, 68 + h:69 + h], lhsT=ktok[par][:, h, :], rhs=ubf[:, gp, c * 4 + h:c * 4 + h + 1],
                            start=True, stop=True),
                            reads=[("ktok", par), ("ubf", gp)], writes=[("small", "dn")], signal=(h == H - 1))
                def dc_pair(pr):
                    for hs in range(2):
                        h = pr * 2 + hs
                        p.op("pe", lambda e, h=h, hs=hs: e.matmul(dC[:, hs, :], lhsT=ktok[par][:, h, :], rhs=vt[par][:, h, :],
                                                                  start=True, stop=True),
                             reads=[("ktok", par), ("vt", par, h // 2)], writes=[("dC",)], signal=(hs == 1))
                    for hs in range(2):
                        h = pr * 2 + hs
                        if first:
                            p.op("dve", lambda e, h=h, hs=hs: e.tensor_copy(out=R32[:, h, :], in_=dC[:, hs, :]),
                                 reads=[("dC",)], writes=[("R32",)])
                        else:
                            p.op("dve", lambda e, h=h, hs=hs: e.scalar_tensor_tensor(
                                out=R32[:, h, :], in0=R32[:, h, :], scalar=egprev[:, h:h + 1], in1=dC[:, hs, :],
                                op0=ALU.mult, op1=ALU.add),
                                reads=[("R32",), ("dC",), egprev_key], writes=[("R32",)])
                dc_pair(0)
                if state.get("s6"):
                    state["s6"]()
                    state["s6"] = None
                dc_pair(1)
                if is_m:
                    ebi = sm[:, 10 + gp, c * 4:(c + 1) * 4]
                    tt_ = [tiny[:, i, :] for i in range(8)]
                    tk = ("tiny",)
                    p.op("dve", lambda e: e.tensor_tensor(out=tt_[0], in0=small[:, 64:68], in1=ebi, op=ALU.max),
                         reads=[("small", "den"), ("sm", 10 + gp)], writes=[tk])
                    p.op("dve", lambda e: e.scalar_tensor_tensor(out=tt_[1], in0=small[:, 64:68], scalar=-1.0, in1=tt_[0],
                                                                 op0=ALU.mult, op1=ALU.max),
                         reads=[("small", "den"), tk], writes=[tk])
                    p.op("dve", lambda e: e.reciprocal(out=tt_[5], in_=tt_[1]), reads=[tk], writes=[tk])
                    scl = tt_[5]
                p.op("pool", lambda e: e.tensor_tensor(
                    out=Cbf[:], in0=R32[:], in1=egcur.unsqueeze(2).broadcast_to([128, H, DV]), op=ALU.mult),
                    reads=[("R32",), egcur_key], writes=[("Cbf",)])
                if is_m:
                    if first:
                        p.op("dve", lambda e: e.tensor_copy(out=n32[:], in_=small[:, 68:72]),
                             reads=[("small", "dn")], writes=[("n32",)])
                    else:
                        p.op("dve", lambda e: e.tensor_tensor(out=n32[:], in0=n32[:], in1=egprev, op=ALU.mult),
                             reads=[("n32",), egprev_key], writes=[("n32",)])
                        p.op("dve", lambda e: e.tensor_tensor(out=n32[:], in0=small[:, 68:72], in1=n32[:], op=ALU.add),
                             reads=[("n32",), ("small", "dn")], writes=[("n32",)])
                    p.op("pool", lambda e: e.tensor_tensor(out=nbf[:], in0=n32[:], in1=egcur, op=ALU.mult),
                         reads=[("n32",), egcur_key], writes=[("nbf",)])
                ss4, sd4, rs4 = tiny[:, 8, :], tiny[:, 9, :], tiny[:, 10, :]
                tk2 = ("tiny2",)
                for h in range(H):
                    pr, hs = h // 2, h % 2
                    if is_m:
                        p.op("dve", lambda e, h=h, pr=pr, hs=hs: e.scalar_tensor_tensor(
                            out=hg[:, h, :], in0=numP[pr][:, hs, :], scalar=scl[:, h:h + 1],
                            in1=og[par][:, h * DV:(h + 1) * DV], op0=ALU.mult, op1=ALU.mult),
                            reads=[numK[pr], ("tiny",), ("og", par, h // 2)], writes=[("hg", h)])
                        p.op("act", lambda e, h=h: e.activation(out=junk[:], in_=hg[:, h, :], func=AF.Square,
                                                                accum_out=ss4[:, h:h + 1]),
                             reads=[("hg", h)], writes=[("junk",), tk2])
                    else:
                        p.op("act", lambda e, h=h, pr=pr, hs=hs: e.activation(out=junk[:], in_=numP[pr][:, hs, :], func=AF.Square,
                                                                              accum_out=ss4[:, h:h + 1]),
                             reads=[numK[pr]], writes=[("junk",), tk2])
                p.op("act", lambda e: e.activation(out=sd4, in_=ss4, func=AF.Sqrt, scale=1.0 / DV, bias=EPS),
                     reads=[tk2], writes=[tk2])
                p.op("dve", lambda e: e.reciprocal(out=rs4, in_=sd4), reads=[tk2], writes=[tk2])
                for h in range(H):
                    pr, hs = h // 2, h % 2
                    if is_m:
                        p.op("pool", lambda e, h=h: e.tensor_scalar(
                            out=hn[:, h, :], in0=hg[:, h, :], scalar1=rs4[:, h:h + 1], scalar2=1.0,
                            op0=ALU.mult, op1=ALU.mult),
                            reads=[("hg", h), tk2], writes=[("hn", h // 2)])
                    else:
                        p.op("dve", lambda e, h=h, pr=pr, hs=hs: e.scalar_tensor_tensor(
                            out=hn[:, h, :], in0=numP[pr][:, hs, :], scalar=rs4[:, h:h + 1],
                            in1=og[par][:, h * DV:(h + 1) * DV], op0=ALU.mult, op1=ALU.mult),
                            reads=[numK[pr], tk2, ("og", par, h // 2)], writes=[("hn", h // 2)])
                def s6():
                    for i in range(8):
                        h, e2 = i // 2, i % 2
                        p.op("pe", lambda e, h=h, e2=e2, i=i: e.transpose(
                            tpS[:, i, :], in_=hn[:, h, e2 * 128:(e2 + 1) * 128], identity=ident[:]),
                            reads=[("hn", h // 2), ("ident",)], writes=[("sc",)], signal=(i == 7))
                    p.op("act", lambda e: e.activation(out=hnT[gp][:, :, cs], in_=tpS[:, :, :], func=AF.Copy),
                         reads=[("sc",)], writes=[("hnT", gp, 0, c), ("hnT", gp, 1, c)])
                state["s6"] = s6
                pend[0:0] = after_s3
                inter(len(pend))
                state["first"] = False

            for st_ in stats_steps(0) + preamble_steps(0) + proj_steps(0, 0):
                st_()
            pend = []
            for g in range(NG):
                for c in range(4):
                    after_s3 = []
                    if c < 3:
                        pend += proj_steps(g, c + 1)
                        if c == 2 and g + 1 < NG:
                            pend += stats_steps(g + 1)
                    else:
                        pre = (preamble_steps(g + 1) + proj_steps(g + 1, 0)) if g + 1 < NG else []
                        opj = outproj_steps(g - 1) if g > 0 else []
                        pend += opj[:4]
                        opj = opj[4:]
                        while pre or opj:
                            if opj:
                                after_s3.append(opj.pop(0))
                            if pre:
                                after_s3.append(pre.pop(0))
                    core_chunk(g, c, pend, after_s3)
                    while pend:
                        pend.pop(0)()
            state["s6"]()
            state["s6"] = None
            for st_ in outproj_steps(NG - 1):
                st_()
            p.barrier()

    def ffn(layer):
        gcol = 16 + layer * 8
        with ExitStack() as ph:
            xn = sb(ph, "xnf", [128, KC, T], BF16)
            hid = sb(ph, "hid", [128, 16, T], BF16)
            w1b = [sb(ph, "w1b", [128, KC, 512], BF16) for _ in range(2)]
            w2b = [sb(ph, "w2b", [128, 16, 256], BF16) for _ in range(2)]
            sq = sb(ph, "sq", [128, 2, GS], BF16)
            sd = sb(ph, "sd", [128, GS], F32)
            rstd = sb(ph, "rstd", [128, GS], F32)
            rt = sb(ph, "rt", [128, 2, GS], F32)
            if dbg == 'mem':
                print('SBUF free in ffn phase', nc.sbuf_bytes_remaining)
            w1src = w1_d[layer].rearrange("(k p) n -> p k n", p=128)
            loads = []
            for hh in range(2):
                for mb in range(4):
                    loads.append(("w1", hh, mb))
                for jb in range(4):
                    loads.append(("w2", hh, jb))
            slot_of = {}
            cnt = dict(w1=0, w2=0)

            def issue(ld):
                kind_, hh, b = ld
                if kind_ == "w1":
                    sl = cnt["w1"] % 2
                    cnt["w1"] += 1
                    col0 = hh * 2048 + b * 512
                    p.dma("pool", "w1b%d" % sl, w1b[sl][:], w1src[:, :, col0:col0 + 512], writes=[("w1b", sl)])
                else:
                    sl = cnt["w2"] % 2
                    cnt["w2"] += 1
                    src = w2_d[layer, hh * 2048:(hh + 1) * 2048, :].rearrange("(m p) n -> p m n", p=128)
                    p.dma("pool", "w2b%d" % sl, w2b[sl][:], src[:, :, b * 256:(b + 1) * 256], writes=[("w2b", sl)])
                slot_of[ld] = sl

            li = 0
            issue(loads[0])
            issue(loads[1])
            li = 2
            for tg in range(NG):
                norm_stats(tg, sq, sd, rstd)
                for k in range(KC):
                    p.op("dve", lambda e, k=k, tg=tg: e.scalar_tensor_tensor(
                        out=xn[:, k, tgs(tg)], in0=xT[:, k, tgs(tg)], scalar=gains[:, gcol + k:gcol + k + 1],
                        in1=rstd[:], op0=ALU.mult, op1=ALU.mult),
                        reads=[("xT", k, tg), ("gains",), ("rstd",)], writes=[("xnf", k, tg)])
            for bi, ld in enumerate(loads):
                kind_, hh, b = ld
                sl = slot_of[ld]
                if kind_ == "w1":
                    for tg in range(NG):
                        for mi in range(4):
                            m = b * 4 + mi
                            a = nxt("acc", 4)
                            for k in range(KC):
                                p.op("pe", lambda e, k=k, tg=tg, a=a, mi=mi, sl=sl: e.matmul(
                                    acc[a][:], lhsT=w1b[sl][:, k, mi * 128:(mi + 1) * 128], rhs=xn[:, k, tgs(tg)],
                                    start=(k == 0), stop=(k == KC - 1)),
                                    reads=[("w1b", sl), ("xnf", k, tg)], writes=[("acc", a)], signal=(k == KC - 1))
                            r2 = nxt("rt", 2)
                            p.op("act", lambda e, a=a, r2=r2: e.activation(out=rt[:, r2, :], in_=acc[a][:], func=AF.Relu),
                                 reads=[("acc", a)], writes=[("rt", r2)])
                            p.op("dve", lambda e, m=m, tg=tg, r2=r2: e.tensor_tensor(
                                out=hid[:, m, tgs(tg)], in0=rt[:, r2, :], in1=rt[:, r2, :], op=ALU.mult),
                                reads=[("rt", r2)], writes=[("hid", m, tg)])
                else:
                    for ji in range(2):
                        j = b * 2 + ji
                        for tg in range(NG):
                            a = nxt("acc", 4)
                            for m in range(16):
                                p.op("pe", lambda e, m=m, tg=tg, a=a, ji=ji, sl=sl: e.matmul(
                                    acc[a][:], lhsT=w2b[sl][:, m, ji * 128:(ji + 1) * 128], rhs=hid[:, m, tgs(tg)],
                                    start=(m == 0), stop=(m == 15)),
                                    reads=[("w2b", sl), ("hid", m, tg)], writes=[("acc", a)], signal=(m == 15))
                            p.op("dve", lambda e, j=j, tg=tg, a=a: e.tensor_tensor(
                                out=xT[:, j, tgs(tg)], in0=acc[a][:], in1=xT[:, j, tgs(tg)], op=ALU.add),
                                reads=[("acc", a), ("xT", j, tg)], writes=[("xT", j, tg)])
                if li < len(loads):
                    issue(loads[li])
                    li += 1
            p.barrier()

    stages = [("mix0", lambda: mixer(0, "mlstm")), ("ffn0", lambda: ffn(0)),
              ("mix1", lambda: mixer(1, "gla")), ("ffn1", lambda: ffn(1))]
    for sname, fn in stages:
        fn()
        if dbg == sname:
            return dump_and_finish()

    with ExitStack() as ph:
        sq = sb(ph, "sq", [128, 2, GS], BF16)
        sd = sb(ph, "sd", [128, GS], F32)
        rstd = sb(ph, "rstd", [128, GS], F32)
        oT = sb(ph, "oT", [128, 2, KC, GS], F32)
        out_v = out_d.rearrange("(k p) t -> p k t", p=128)
        for tg in range(NG):
            norm_stats(tg, sq, sd, rstd)
            o2 = tg % 2
            for k in range(KC):
                p.op("dve", lambda e, k=k, tg=tg, o2=o2: e.scalar_tensor_tensor(
                    out=oT[:, o2, k, :], in0=xT[:, k, tgs(tg)], scalar=gains[:, 32 + k:33 + k],
                    in1=rstd[:], op0=ALU.mult, op1=ALU.mult),
                    reads=[("xT", k, tg), ("gains",), ("rstd",)], writes=[("oT", o2)])
            p.dma("sp", "out%d" % o2, out_v[:, :, tgs(tg)], oT[:, o2, :, :], reads=[("oT", o2)])
        for o2 in range(2):
            d = p.dsem["out%d" % o2]
            nc.sync.wait_ge(d[0], d[1])
    es.close()
    return nc


def prep_inputs(inp):
    f = lambda a: np.ascontiguousarray(np.asarray(a, dtype=np.float32))
    nm = f(inp["norm_mix_g"]).reshape(2, 8, 128).transpose(2, 0, 1).reshape(128, 16)
    nf = f(inp["norm_ffn_g"]).reshape(2, 8, 128).transpose(2, 0, 1).reshape(128, 16)
    fg = f(inp["final_norm_g"]).reshape(8, 128).T
    gains = f(np.concatenate([nm, nf, fg], axis=1))
    shared = {
        "gains": gains,
        "m_win": f(inp["mlstm_w_in"][0]),
        "m_bg": f(inp["mlstm_b_gate"]).reshape(1, 8),
        "m_ong": f(inp["mlstm_out_norm_g"]).reshape(1, 1024),
        "m_ongp": f(f(inp["mlstm_out_norm_g"]).reshape(8, 128).T),
        "m_wout": f(inp["mlstm_w_out"][0]),
        "g_win": f(inp["gla_w_in"][0]),
        "g_wgu": f(inp["gla_w_gate_up"][0]),
        "g_bg": f(f(inp["gla_b_gate"]).reshape(4, 128).T),
        "g_ong": f(inp["gla_out_norm_g"]).reshape(1, 1024),
        "g_wout": f(inp["gla_w_out"][0]),
        "w1": f(inp["ffn_w1"]),
        "w2": f(inp["ffn_w2"]),
    }
    x = np.asarray(inp["x"], dtype=np.float32)
    in_maps = []
    for b in range(8):
        m = dict(shared)
        m["xT"] = np.ascontiguousarray(x[b].T)
        in_maps.append(m)
    return in_maps


def kernel(**inputs):
    in_maps = prep_inputs(inputs)
    nc = build()
    res = run_bass_kernel_spmd(nc, in_maps, core_ids=list(range(8)))
    out = np.stack([np.ascontiguousarray(res.results[b]["outT"].T) for b in range(8)], axis=0)
    return out.astype(np.float32)
```

```python
import numpy as np
from contextlib import ExitStack
import concourse.bass as bass
import concourse.mybir as mybir
from concourse.bass_utils import run_bass_kernel_spmd

F32 = mybir.dt.float32
BF16 = mybir.dt.bfloat16
AF = mybir.ActivationFunctionType
ALU = mybir.AluOpType

T = 2048
D = 1024
KC = 8
NG = 4
GS = 512
CH = 128
H = 4
DK = 128
DV = 256
DFF = 4096
EPS = 1e-6
QSCALE = DK ** -0.5


class P:
    def __init__(s, nc, es):
        s.nc = nc
        s.es = es
        s.E = {}
        for name, eng in (("pe", nc.tensor), ("act", nc.scalar), ("dve", nc.vector),
                          ("pool", nc.gpsimd), ("sp", nc.sync)):
            s.E[name] = dict(eng=eng, sem=es.enter_context(nc.semaphore("s_" + name)), count=0, waited={})
        s.lastw = {}
        s.readers = {}
        s.dsem = {}
        s.uid = 0

    def name(s, base):
        s.uid += 1
        return "%s_%d" % (base, s.uid)

    def _waits(s, ename, reads, writes):
        e = s.E[ename]
        pesem = s.E["pe"]["sem"]
        need = {}

        def add(sem, val):
            if ename == "pe" and sem is pesem:
                return
            if need.get(sem, 0) < val:
                need[sem] = val
        for k in reads:
            t = s.lastw.get(k)
            if t:
                add(*t)
        for k in writes:
            t = s.lastw.get(k)
            if t:
                add(*t)
            for sem, val in s.readers.get(k, {}).items():
                add(sem, val)
        for sem, val in need.items():
            if e["waited"].get(sem, 0) < val:
                e["eng"].wait_ge(sem, val)
                e["waited"][sem] = val

    def _reg(s, tok, reads, writes):
        for k in reads:
            d = s.readers.setdefault(k, {})
            if d.get(tok[0], 0) < tok[1]:
                d[tok[0]] = tok[1]
        for k in writes:
            s.lastw[k] = tok
            s.readers[k] = {}

    @staticmethod
    def _split(reads, writes):
        def nk(k):
            if k[0] in ("sc", "num", "dC"):
                return (k[0],), True
            if k[0] in ("tp", "small"):
                return ("misc",), True
            if k[0] == "acc":
                return k, True
            return k, False
        r2, w2 = [], []
        for k in reads:
            k2, ex = nk(k)
            (w2 if ex else r2).append(k2)
        for k in writes:
            w2.append(nk(k)[0])
        return r2, w2

    def op(s, ename, fn, reads=(), writes=(), signal=True):
        reads, writes = s._split(reads, writes)
        s._waits(ename, reads, writes)
        e = s.E[ename]
        ins = fn(e["eng"])
        if signal:
            e["count"] += 1
            ins.then_inc(e["sem"], 1)
            tok = (e["sem"], e["count"])
        else:
            tok = (e["sem"], e["count"] + 1)
        s._reg(tok, reads, writes)

    def dma(s, qname, dname, out, in_=None, reads=(), writes=()):
        pairs = out if in_ is None else [(out, in_)]
        s._waits(qname, reads, writes)
        if dname not in s.dsem:
            s.dsem[dname] = [s.es.enter_context(s.nc.semaphore("d_" + dname)), 0]
        d = s.dsem[dname]
        for o, i in pairs:
            d[1] += 16
            s.E[qname]["eng"].dma_start(out=o, in_=i).then_inc(d[0], 16)
        s._reg((d[0], d[1]), reads, writes)
        return (d[0], d[1])

    def barrier(s):
        toks = [(e["sem"], e["count"]) for e in s.E.values() if e["count"] > 0]
        toks += [(d[0], d[1]) for d in s.dsem.values()]
        pesem = s.E["pe"]["sem"]
        for name, e in s.E.items():
            for sem, val in toks:
                if name == "pe" and sem is pesem:
                    continue
                if e["waited"].get(sem, 0) < val:
                    e["eng"].wait_ge(sem, val)
                    e["waited"][sem] = val


def build(dbg=None):
    nc = bass.Bass("TRN2", target_bir_lowering=False)

    def dram(name, shape, kind="ExternalInput"):
        return nc.dram_tensor(name, list(shape), F32, kind=kind).ap()

    xT_d = dram("xT", [D, T])
    gains_d = dram("gains", [128, 40])
    m_win_d = dram("m_win", [D, 3080])
    m_bg_d = dram("m_bg", [1, 8])
    m_ong_d = dram("m_ong", [1, 1024])
    m_wout_d = dram("m_wout", [D, D])
    g_win_d = dram("g_win", [D, 3088])
    g_wgu_d = dram("g_wgu", [16, 512])
    g_bg_d = dram("g_bg", [128, 4])
    g_ong_d = dram("g_ong", [1, 1024])
    g_wout_d = dram("g_wout", [D, D])
    w1_d = dram("w1", [2, D, DFF])
    w2_d = dram("w2", [2, DFF, D])
    out_d = dram("outT", [D, T], kind="ExternalOutput")

    es = ExitStack()
    p = P(nc, es)

    def sb(stack, base, shape, dt):
        return stack.enter_context(nc.sbuf_tensor(p.name(base), list(shape), dt))

    def ps(base, shape, dt):
        return es.enter_context(nc.psum_tensor(p.name(base), list(shape), dt))

    xT = sb(es, "xT", [128, KC, T], F32)
    gains = sb(es, "gains", [128, 40], F32)
    ident = sb(es, "ident", [128, 128], BF16)
    tri_f = sb(es, "tri_f", [128, 128], F32)
    ones_f = sb(es, "ones_f", [128, 128], F32)
    ones_bf = sb(es, "ones_bf", [128, 128], BF16)
    rmask = sb(es, "rmask", [128, GS], F32)

    acc = [ps("acc", [128, 512], F32) for _ in range(4)]
    sc = ps("sc", [128, 4, 128], F32)
    num = ps("num", [128, 2, 256], F32)
    dC = ps("dC", [128, 2, 256], F32)
    tpsm = ps("tpsm", [128, 512], F32)
    tp = tpsm[:, 0:256].bitcast(BF16).rearrange("p (s n) -> p s n", n=128)
    small = tpsm[:, 256:512]
    rr = dict(accm=0, acc=0, sc=0, num=0, dC=0, tp=0, den=0, rt=0)

    def nxt(nm, n):
        i = rr[nm] % n
        rr[nm] = (i + 1) % n
        return i

    def emit_consts():
        p.op("dve", lambda e: e.memset(ident[:], 0.0), writes=[("ident",)])
        p.op("dve", lambda e: e.memset(ones_f[:], 1.0), writes=[("ones_f",)])
        p.op("dve", lambda e: e.memset(ones_bf[:], 1.0), writes=[("ones_bf",)])
        p.op("dve", lambda e: e.memset(tri_f[:], 1.0), writes=[("tri_f",)])
        p.op("dve", lambda e: e.memset(rmask[:], 1.0), writes=[("rmask",)])
        p.op("dve", lambda e: e.memset(rmask[:].rearrange("p (c t) -> p c t", t=CH)[:, :, 0:1], 0.0),
             reads=[("rmask",)], writes=[("rmask",)])
        p.op("pool", lambda e: e.affine_select(out=ident[:], in_=ident[:], compare_op=ALU.not_equal, fill=1.0,
                                               base=0, pattern=[[-1, 128]], channel_multiplier=1),
             reads=[("ident",)], writes=[("ident",)])
        p.op("pool", lambda e: e.affine_select(out=tri_f[:], in_=tri_f[:], compare_op=ALU.is_ge, fill=0.0,
                                               base=0, pattern=[[1, 128]], channel_multiplier=-1),
             reads=[("tri_f",)], writes=[("tri_f",)])

    p.dma("sp", "gains", gains[:], gains_d, writes=[("gains",)])
    xT_v = xT_d.rearrange("(k p) t -> p k t", p=128)
    def load_xT(q, tg):
        p.dma(q, "xT%d" % tg, [(xT[:, k0:k0 + 4, tg * GS:(tg + 1) * GS], xT_v[:, k0:k0 + 4, tg * GS:(tg + 1) * GS])
                               for k0 in (0, 4)],
              writes=[("xT", k, tg) for k in range(KC)])
    load_xT("sp", 0)

    def tgs(tg):
        return slice(tg * GS, (tg + 1) * GS)

    def norm_stats(tg, sq, sd, rstd, banks=None):
        a = nxt("acc", 4) if banks is None else banks[nxt("accm", len(banks))]
        for k in range(KC):
            sl = k % 2
            p.op("act", lambda e, k=k, sl=sl: e.activation(out=sq[:, sl, :], in_=xT[:, k, tgs(tg)], func=AF.Square),
                 reads=[("xT", k, tg)], writes=[("sq", sl)])
            p.op("pe", lambda e, k=k, sl=sl: e.matmul(acc[a][:], lhsT=ones_bf[:], rhs=sq[:, sl, :],
                                                     start=(k == 0), stop=(k == KC - 1)),
                 reads=[("sq", sl), ("ones_bf",)], writes=[("acc", a)], signal=True)
        p.op("act", lambda e: e.activation(out=sd[:], in_=acc[a][:], func=AF.Ln, scale=1.0 / D, bias=EPS),
             reads=[("acc", a)], writes=[("sd",)])
        p.op("act", lambda e: e.activation(out=rstd[:], in_=sd[:], func=AF.Exp, scale=-0.5), reads=[("sd",)], writes=[("rstd",)])

    def dump_and_finish():
        p.dma("sp", "out", [(out_d[k * 128:(k + 1) * 128, :], xT[:, k, :]) for k in range(KC)],
              reads=[("xT", k, tg) for k in range(KC) for tg in range(NG)])
        d = p.dsem["out"]
        nc.sync.wait_ge(d[0], d[1])
        es.close()
        return nc

    def mixer(layer, kind):
        is_m = (kind == "mlstm")
        win_d = m_win_d if is_m else g_win_d
        wout_d = m_wout_d if is_m else g_wout_d
        ong_d = m_ong_d if is_m else g_ong_d
        NIN = 3080 if is_m else 3088
        gcol = layer * 8
        numP = [num, acc[2][:].rearrange("p (s n) -> p s n", n=DV)]
        numK = [("num",), ("acc", 2)]
        dCP = [dC, dC]
        dCK = [("dC",), ("dC",)]
        PB = [0, 1, 3]
        tpS = sc[:].rearrange("p s n -> p (s n)").bitcast(BF16).rearrange("p (s n) -> p s n", n=128)
        tpK = dC[:].rearrange("p s n -> p (s n)").bitcast(BF16).rearrange("p (s n) -> p s n", n=128)
        with ExitStack() as ph:
            win = sb(ph, "win", [128, KC, NIN], BF16)
            wout = sb(ph, "wout", [128, KC, D], BF16)
            ong_b = sb(ph, "ong_b", [128, 1024], F32)
            xn = sb(ph, "xn", [128, KC, GS], BF16)
            sq = sb(ph, "sq", [128, 2, GS], BF16)
            sd = sb(ph, "sd", [128, GS], F32)
            rstd = sb(ph, "rstd", [128, GS], F32)
            qT = sb(ph, "qT", [128, H, GS], BF16)
            kT = sb(ph, "kT", [128, H, GS], BF16)
            ktok = [sb(ph, "ktok", [128, H, DK], BF16) for _ in range(2)]
            vt = [sb(ph, "vt", [128, H, DV], BF16) for _ in range(2)]
            og = [sb(ph, "og", [128, H * DV], F32) for _ in range(2)]
            St = sb(ph, "St", [128, H, 128], BF16)
            tri4 = sb(ph, "tri4", [128, H, 128], BF16)
            junk = sb(ph, "junk", [128, DV], BF16)
            hn = sb(ph, "hn", [128, H, DV], BF16)
            hnT = [sb(ph, "hnT", [128, KC, GS], BF16) for _ in range(2)]
            R32 = sb(ph, "R32", [128, H, DV], F32)
            Cbf = sb(ph, "Cbf", [128, H, DV], BF16)
            sm = sb(ph, "sm", [128, 16, 16], F32)
            egs = sb(ph, "egs", [128, 2, 16], F32)
            tiny = sb(ph, "tiny", [128, 16, 4], F32)
            if is_m:
                hg = sb(ph, "hg", [128, H, DV], F32)
                n32 = sb(ph, "n32", [128, H], F32)
                nbf = sb(ph, "nbf", [128, H], BF16)
                ubf = sb(ph, "ubf", [128, 2, 16], BF16)
                bg4 = sb(ph, "bg4", [128, 4, 8], F32)
                gates = sb(ph, "gates", [128, 4, 8], F32)
            else:
                wgu = sb(ph, "wgu", [16, 512], F32)
                gbg = sb(ph, "gbg", [128, 4], F32)
                ngbg = sb(ph, "ngbg", [128, 4], F32)
                zl = sq[0:16, :, :].rearrange("p a t -> p (a t)").bitcast(F32)
                e1 = sd[:].rearrange("p (o t) -> p o t", o=1)
                nbT = rstd[:].rearrange("p (o t) -> p o t", o=1)
                EB = sb(ph, "EB", [128, 1, GS], F32)
                EK = sb(ph, "EK", [128, 1, GS], F32)

            if dbg == 'mem':
                print('SBUF free in mixer phase', kind, nc.sbuf_bytes_remaining)
            win_src = win_d.rearrange("(k p) n -> p k n", p=128)
            p.dma("pool", "winZ", [(win[:, :, 3072:NIN], win_src[:, :, 3072:NIN])], writes=[("win", "gate")])
            p.dma("pool", "winA", [(win[:, k0:k0 + 4, 0:1024], win_src[:, k0:k0 + 4, 0:1024]) for k0 in range(0, KC, 4)],
                  writes=[("win", "qk")])
            p.dma("pool", "winB", [(win[:, k0:k0 + 2, 1024:3072], win_src[:, k0:k0 + 2, 1024:3072]) for k0 in range(0, KC, 2)],
                  writes=[("win", "rest")])
            if layer == 0:
                for tg in range(1, NG):
                    load_xT("pool", tg)
                emit_consts()
            wout_src = wout_d.rearrange("(k p) n -> p k n", p=128)
            p.dma("pool", "wout", [(wout[:, k0:k0 + 4, :], wout_src[:, k0:k0 + 4, :]) for k0 in range(0, KC, 4)],
                  writes=[("wout",)])
            if is_m:
                p.dma("sp", "misc", [(ong_b[:], ong_d.partition_broadcast(128))] +
                      [(bg4[:, c, :], m_bg_d.partition_broadcast(128)) for c in range(4)],
                      writes=[("ong_b",), ("bg4",)])
            else:
                p.dma("sp", "misc", [(ong_b[:], ong_d.partition_broadcast(128)), (wgu[:], g_wgu_d), (gbg[:], g_bg_d)],
                      writes=[("ong_b",), ("wgu",), ("gbg",)])
                p.op("dve", lambda e: e.tensor_scalar(out=ngbg[:], in0=gbg[:], scalar1=-1.0, scalar2=None,
                                                      op0=ALU.mult), reads=[("gbg",)], writes=[("ngbg",)])
            for h in range(H):
                p.op("pool", lambda e, h=h: e.tensor_copy(out=tri4[:, h, :], in_=tri_f[:]), reads=[("tri_f",)],
                     writes=[("tri4",)])

            state = dict(first=True)

            def proj_fm(col0, M, a):
                for k in range(KC):
                    p.op("pe", lambda e, k=k: e.matmul(acc[a][0:M, :], lhsT=win[:, k, col0:col0 + M],
                                                      rhs=xn[:, k, :], start=(k == 0), stop=(k == KC - 1)),
                         reads=[("win", "qk" if col0 < 1024 else ("gate" if col0 >= 3072 else "rest")), ("xn", k)], writes=[("acc", a)], signal=(k == KC - 1))

            def proj_tm(c, col0, N, a):
                for k in range(KC):
                    p.op("pe", lambda e, k=k: e.matmul(acc[a][:, 0:N], lhsT=xn[:, k, c * CH:(c + 1) * CH],
                                                      rhs=win[:, k, col0:col0 + N],
                                                      start=(k == 0), stop=(k == KC - 1)),
                         reads=[("win", "rest"), ("xn", k)], writes=[("acc", a)], signal=(k == KC - 1))

            c4 = lambda ap: ap.rearrange("p (c h) -> p c h", h=4)

            def stats_steps(g):
                return [lambda: norm_stats(g, sq, sd, rstd, banks=PB)]

            def preamble_steps(g):
                gp = g % 2
                steps = []

                def s_xn():
                    for k in range(KC):
                        p.op("dve", lambda e, k=k: e.scalar_tensor_tensor(
                            out=xn[:, k, :], in0=xT[:, k, tgs(g)], scalar=gains[:, gcol + k:gcol + k + 1],
                            in1=rstd[:], op0=ALU.mult, op1=ALU.mult),
                            reads=[("xT", k, g), ("gains",), ("rstd",)], writes=[("xn", k)])
                steps.append(s_xn)

                def s_q(h):
                    a = PB[nxt("accm", 3)]
                    proj_fm(h * DK, DK, a)
                    if is_m:
                        p.op("act", lambda e: e.activation(out=qT[:, h, :], in_=acc[a][:], func=AF.Copy),
                             reads=[("acc", a)], writes=[("qT", h)])
                    else:
                        p.op("dve", lambda e: e.scalar_tensor_tensor(
                            out=qT[:, h, :], in0=acc[a][:], scalar=QSCALE, in1=EB[:, 0, :], op0=ALU.mult, op1=ALU.mult),
                            reads=[("acc", a), ("EB",)], writes=[("qT", h)])

                def s_k(h):
                    a = PB[nxt("accm", 3)]
                    proj_fm(512 + h * DK, DK, a)
                    if is_m:
                        p.op("act", lambda e: e.activation(out=kT[:, h, :], in_=acc[a][:], func=AF.Copy, scale=QSCALE),
                             reads=[("acc", a)], writes=[("kT", h)])
                    else:
                        p.op("dve", lambda e: e.tensor_tensor(
                            out=kT[:, h, :], in0=acc[a][:], in1=EK[:, 0, :], op=ALU.mult),
                            reads=[("acc", a), ("EK",)], writes=[("kT", h)])

                if is_m:
                    ef, spt, tmpu = sm[:, 0, :], sm[:, 1, :], sm[:, 2, :]
                    u, eb = sm[:, 8 + gp, :], sm[:, 10 + gp, :]

                    def s_gates_a():
                        for c in range(4):
                            for k in range(KC):
                                p.op("pe", lambda e, k=k, c=c: e.matmul(
                                    small[:, c * 8:(c + 1) * 8], lhsT=xn[:, k, c * CH:(c + 1) * CH],
                                    rhs=win[:, k, 3072:3080], start=(k == 0), stop=(k == KC - 1)),
                                    reads=[("win", "gate"), ("xn", k)], writes=[("small", "gates")], signal=(k == KC - 1))
                        p.op("dve", lambda e: e.tensor_tensor(
                            out=gates[:], in0=small[:, 0:32].rearrange("p (c n) -> p c n", n=8), in1=bg4[:], op=ALU.add),
                            reads=[("small", "gates"), ("bg4",)], writes=[("gates",)])
                        p.op("act", lambda e: e.activation(out=c4(ef), in_=gates[:, :, 4:8], func=AF.Exp, scale=-1.0),
                             reads=[("gates",)], writes=[("sm", 0)])
                        p.op("act", lambda e: e.activation(out=spt, in_=ef, func=AF.Ln, bias=1.0),
                             reads=[("sm", 0)], writes=[("sm", 1)])

                    def s_gates_b():
                        p.op("pe", lambda e: e.matmul(small[:, 32:48], lhsT=tri_f[:], rhs=spt, start=True, stop=True),
                             reads=[("tri_f",), ("sm", 1)], writes=[("small", "bs")])
                        p.op("pe", lambda e: e.matmul(small[:, 48:64], lhsT=ones_f[:], rhs=spt, start=True, stop=True),
                             reads=[("ones_f",), ("sm", 1)], writes=[("small", "gs")])
                        p.op("dve", lambda e: e.tensor_tensor(out=c4(tmpu), in0=c4(small[:, 32:48]), in1=gates[:, :, 0:4],
                                                              op=ALU.add),
                             reads=[("small", "bs"), ("gates",)], writes=[("sm", 2)])
                        p.op("act", lambda e: e.activation(out=u, in_=tmpu, func=AF.Exp), reads=[("sm", 2)],
                             writes=[("sm", 8 + gp)])
                        p.op("act", lambda e: e.activation(out=eb, in_=small[:, 32:48], func=AF.Exp),
                             reads=[("small", "bs")], writes=[("sm", 10 + gp)])
                        p.op("act", lambda e: e.activation(out=egs[:, gp, :], in_=small[:, 48:64], func=AF.Exp, scale=-1.0),
                             reads=[("small", "gs")], writes=[("egs", gp)])
                        p.op("dve", lambda e: e.tensor_copy(out=ubf[:, gp, :], in_=u), reads=[("sm", 8 + gp)],
                             writes=[("ubf", gp)])
                    steps += [s_gates_a, lambda: s_q(0), lambda: s_k(0), s_gates_b]
                    for h in range(1, H):
                        steps += [lambda h=h: s_q(h), lambda h=h: s_k(h)]
                else:
                    def s_zl():
                        a = PB[nxt("accm", 3)]
                        proj_fm(3072, 16, a)
                        p.op("dve", lambda e: e.tensor_copy(out=zl, in_=acc[a][0:16, :]),
                             reads=[("acc", a)], writes=[("sq", 0), ("sq", 1)])

                    def s_z1(h):
                        a = PB[nxt("accm", 3)]
                        p.op("pe", lambda e: e.matmul(acc[a][:], lhsT=wgu[:, h * DK:(h + 1) * DK], rhs=zl,
                                                      start=True, stop=True),
                             reads=[("wgu",), ("sq", 0), ("sq", 1)], writes=[("acc", a)])
                        p.op("act", lambda e: e.activation(out=e1[:, 0, :], in_=acc[a][:], func=AF.Exp,
                                                           scale=-1.0, bias=ngbg[:, h:h + 1]),
                             reads=[("acc", a), ("ngbg",)], writes=[("sd",)])
                        p.op("act", lambda e: e.activation(out=e1[:, 0, :], in_=e1[:, 0, :], func=AF.Ln, bias=1.0),
                             reads=[("sd",)], writes=[("sd",)])
                        p.op("dve", lambda e: e.tensor_tensor_scan(out=nbT[:, 0, :], data0=rmask[:], data1=e1[:, 0, :],
                                                                  initial=0.0, op0=ALU.mult, op1=ALU.add),
                             reads=[("rmask",), ("sd",)], writes=[("rstd",)])

                    def s_z2(h):
                        p.op("act", lambda e: e.activation(out=EB[:, 0, :], in_=nbT[:, 0, :], func=AF.Exp,
                                                           scale=-1.0 / 16.0),
                             reads=[("rstd",)], writes=[("EB",)])
                        p.op("act", lambda e: e.activation(out=EK[:, 0, :], in_=nbT[:, 0, :], func=AF.Exp, scale=1.0 / 16.0),
                             reads=[("rstd",)], writes=[("EK",)])
                        p.op("dve", lambda e: e.tensor_copy(
                            out=egs[:, gp, h * 4:(h + 1) * 4],
                            in_=EB[:, 0, :].rearrange("p (c t) -> p c t", t=CH)[:, :, CH - 1]),
                            reads=[("EB",)], writes=[("egs", gp)])

                    def s_head(h):
                        a1 = PB[nxt("accm", 3)]
                        proj_fm(h * DK, DK, a1)
                        a2 = PB[nxt("accm", 3)]
                        proj_fm(512 + h * DK, DK, a2)
                        p.op("dve", lambda e: e.scalar_tensor_tensor(
                            out=qT[:, h, :], in0=acc[a1][:], scalar=QSCALE, in1=EB[:, 0, :], op0=ALU.mult, op1=ALU.mult),
                            reads=[("acc", a1), ("EB",)], writes=[("qT", h)])
                        p.op("dve", lambda e: e.tensor_tensor(
                            out=kT[:, h, :], in0=acc[a2][:], in1=EK[:, 0, :], op=ALU.mult),
                            reads=[("acc", a2), ("EK",)], writes=[("kT", h)])
                        if h + 1 < H:
                            s_z1(h + 1)
                            s_z2(h + 1)
                    steps += [s_zl, lambda: (s_z1(0), s_z2(0))]
                    for h in range(H):
                        steps.append(lambda h=h: s_head(h))
                return steps

            def proj_steps(g, c):
                par = c % 2
                gp = g % 2
                steps = []
                for vb in range(2):
                    def s_v(vb=vb):
                        a = PB[nxt("accm", 3)]
                        proj_tm(c, 1024 + vb * 512, 512, a)
                        if is_m:
                            for hh in range(2):
                                h = vb * 2 + hh
                                p.op("act", lambda e, h=h, hh=hh: e.activation(
                                    out=vt[par][:, h, :], in_=acc[a][:, hh * DV:(hh + 1) * DV], func=AF.Identity,
                                    scale=sm[:, 8 + gp, c * 4 + h:c * 4 + h + 1]),
                                    reads=[("acc", a), ("sm", 8 + gp)], writes=[("vt", par, vb)])
                        else:
                            p.op("act", lambda e: e.activation(
                                out=vt[par][:, vb * 2:vb * 2 + 2, :].rearrange("p h d -> p (h d)"), in_=acc[a][:],
                                func=AF.Copy),
                                reads=[("acc", a)], writes=[("vt", par, vb)])
                    steps.append(s_v)
                for vb in range(2):
                    def s_o(vb=vb):
                        a = PB[nxt("accm", 3)]
                        proj_tm(c, 2048 + vb * 512, 512, a)
                        if is_m:
                            p.op("act", lambda e: e.activation(out=og[par][:, vb * 512:(vb + 1) * 512], in_=acc[a][:],
                                                               func=AF.Sigmoid),
                                 reads=[("acc", a)], writes=[("og", par, vb)])
                        else:
                            p.op("act", lambda e: e.activation(out=og[par][:, vb * 512:(vb + 1) * 512], in_=acc[a][:],
                                                               func=AF.Silu),
                                 reads=[("acc", a)], writes=[("og", par, vb)])
                            p.op("pool", lambda e: e.tensor_tensor(
                                out=og[par][:, vb * 512:(vb + 1) * 512], in0=og[par][:, vb * 512:(vb + 1) * 512],
                                in1=ong_b[:, vb * 512:(vb + 1) * 512], op=ALU.mult),
                                reads=[("og", par, vb), ("ong_b",)], writes=[("og", par, vb)])
                    steps.append(s_o)
                return steps

            def outproj_steps(g):
                gp = g % 2
                steps = []
                for j in range(KC):
                    def s_j(j=j):
                        a = PB[nxt("accm", 3)]
                        for ec in range(KC):
                            p.op("pe", lambda e, ec=ec: e.matmul(
                                acc[a][:], lhsT=wout[:, ec, j * 128:(j + 1) * 128], rhs=hnT[gp][:, ec, :],
                                start=(ec == 0), stop=(ec == KC - 1)),
                                reads=[("wout",)] + [("hnT", gp, ec // 4, c) for c in range(4)], writes=[("acc", a)],
                                signal=(ec == KC - 1))
                        p.op("dve", lambda e: e.tensor_tensor(out=xT[:, j, tgs(g)], in0=acc[a][:], in1=xT[:, j, tgs(g)],
                                                              op=ALU.add),
                             reads=[("acc", a), ("xT", j, g)], writes=[("xT", j, g)])
                    steps.append(s_j)
                return steps

            def core_chunk(g, c, pend, after_s3):
                gp = g % 2
                cs = slice(c * CH, (c + 1) * CH)
                par = c % 2
                first = state["first"]

                def inter(n=1):
                    for _ in range(n):
                        if pend:
                            pend.pop(0)()

                if is_m:
                    egprev = (egs[:, gp, (c - 1) * 4:c * 4] if c > 0 else egs[:, 1 - gp, 12:16])
                    egcur = egs[:, gp, c * 4:(c + 1) * 4]
                else:
                    ev = lambda par_: egs[:, par_, :].rearrange("p (h c) -> p h c", c=4)
                    egprev = (ev(gp)[:, :, c - 1] if c > 0 else ev(1 - gp)[:, :, 3])
                    egcur = ev(gp)[:, :, c]
                egprev_key = ("egs", gp) if c > 0 else ("egs", 1 - gp)
                egcur_key = ("egs", gp)
                for h in range(H):
                    p.op("pe", lambda e, h=h: e.matmul(sc[:, h, :], lhsT=kT[:, h, cs], rhs=qT[:, h, cs],
                                                       start=True, stop=True),
                         reads=[("kT", h), ("qT", h)], writes=[("sc",)], signal=(h == H - 1))
                if True:
                    for h in range(H):
                        p.op("pe", lambda e, h=h: e.transpose(tpK[:, h, :], in_=kT[:, h, cs], identity=ident[:]),
                             reads=[("kT", h), ("ident",)], writes=[("dC",)], signal=(h == H - 1))
                    p.op("act", lambda e: e.activation(out=ktok[par][:], in_=tpK[:, 0:4, :], func=AF.Copy),
                         reads=[("dC",)], writes=[("ktok", par)])
                p.op("dve", lambda e: e.tensor_tensor(out=St[:], in0=sc[:, :, :], in1=tri4[:], op=ALU.mult),
                     reads=[("sc",), ("tri4",)], writes=[("St",)])
                inter(len(pend))
                for h in range(H):
                    pr, hs = h // 2, h % 2
                    p.op("pe", lambda e, h=h, pr=pr, hs=hs: e.matmul(numP[pr][:, hs, :], lhsT=St[:, h, :], rhs=vt[par][:, h, :],
                                                                    start=True, stop=first),
                         reads=[("St",), ("vt", par, h // 2)], writes=[numK[pr]], signal=(first and hs == 1))
                    if not first:
                        p.op("pe", lambda e, h=h, pr=pr, hs=hs: e.matmul(numP[pr][:, hs, :], lhsT=qT[:, h, cs], rhs=Cbf[:, h, :],
                                                                        start=False, stop=True),
                             reads=[("qT", h), ("Cbf",)], writes=[numK[pr]], signal=(hs == 1))
                if is_m:
                    for h in range(H):
                        p.op("pe", lambda e, h=h: e.matmul(
                            small[:, 64 + h:65 + h], lhsT=St[:, h, :], rhs=ubf[:, gp, c * 4 + h:c * 4 + h + 1],
                            start=True, stop=first),
                            reads=[("St",), ("ubf", gp)], writes=[("small", "den")], signal=False)
                        if not first:
                            p.op("pe", lambda e, h=h: e.matmul(
                                small[:, 64 + h:65 + h], lhsT=qT[:, h, cs], rhs=nbf[:, h:h + 1],
                                start=False, stop=True),
                                reads=[("qT", h), ("nbf",)], writes=[("small", "den")], signal=False)
                    for h in range(H):
                        p.op("pe", lambda e, h=h: e.matmul(
                            small[:, 68 + h:69 + h], lhsT=ktok[par][:, h, :], rhs=ubf[:, gp, c * 4 + h:c * 4 + h + 1],
                            start=True, stop=True),
                            reads=[("ktok", par), ("ubf", gp)], writes=[("small", "dn")], signal=(h == H - 1))
                def dc_pair(pr):
                    for hs in range(2):
                        h = pr * 2 + hs
                        p.op("pe", lambda e, h=h, hs=hs: e.matmul(dC[:, hs, :], lhsT=ktok[par][:, h, :], rhs=vt[par][:, h, :],
                                                                  start=True, stop=True),
                             reads=[("ktok", par), ("vt", par, h // 2)], writes=[("dC",)], signal=(hs == 1))
                    for hs in range(2):
                        h = pr * 2 + hs
                        if first:
                            p.op("dve", lambda e, h=h, hs=hs: e.tensor_copy(out=R32[:, h, :], in_=dC[:, hs, :]),
                                 reads=[("dC",)], writes=[("R32",)])
                        else:
                            p.op("dve", lambda e, h=h, hs=hs: e.scalar_tensor_tensor(
                                out=R32[:, h, :], in0=R32[:, h, :], scalar=egprev[:, h:h + 1], in1=dC[:, hs, :],
                                op0=ALU.mult, op1=ALU.add),
                                reads=[("R32",), ("dC",), egprev_key], writes=[("R32",)])
                dc_pair(0)
                if state.get("s6"):
                    state["s6"]()
                    state["s6"] = None
                dc_pair(1)
                if is_m:
                    ebi = sm[:, 10 + gp, c * 4:(c + 1) * 4]
                    tt_ = [tiny[:, i, :] for i in range(8)]
                    tk = ("tiny",)
                    p.op("dve", lambda e: e.tensor_tensor(out=tt_[0], in0=small[:, 64:68], in1=ebi, op=ALU.max),
                         reads=[("small", "den"), ("sm", 10 + gp)], writes=[tk])
                    p.op("dve", lambda e: e.scalar_tensor_tensor(out=tt_[1], in0=small[:, 64:68], scalar=-1.0, in1=tt_[0],
                                                                 op0=ALU.mult, op1=ALU.max),
                         reads=[("small", "den"), tk], writes=[tk])
                    p.op("dve", lambda e: e.reciprocal(out=tt_[5], in_=tt_[1]), reads=[tk], writes=[tk])
                    scl = tt_[5]
                p.op("pool", lambda e: e.tensor_tensor(
                    out=Cbf[:], in0=R32[:], in1=egcur.unsqueeze(2).broadcast_to([128, H, DV]), op=ALU.mult),
                    reads=[("R32",), egcur_key], writes=[("Cbf",)])
                if is_m:
                    if first:
                        p.op("dve", lambda e: e.tensor_copy(out=n32[:], in_=small[:, 68:72]),
                             reads=[("small", "dn")], writes=[("n32",)])
                    else:
                        p.op("dve", lambda e: e.tensor_tensor(out=n32[:], in0=n32[:], in1=egprev, op=ALU.mult),
                             reads=[("n32",), egprev_key], writes=[("n32",)])
                        p.op("dve", lambda e: e.tensor_tensor(out=n32[:], in0=small[:, 68:72], in1=n32[:], op=ALU.add),
                             reads=[("n32",), ("small", "dn")], writes=[("n32",)])
                    p.op("pool", lambda e: e.tensor_tensor(out=nbf[:], in0=n32[:], in1=egcur, op=ALU.mult),
                         reads=[("n32",), egcur_key], writes=[("nbf",)])
                ss4, sd4, rs4 = tiny[:, 8, :], tiny[:, 9, :], tiny[:, 10, :]
                tk2 = ("tiny2",)
                for h in range(H):
                    pr, hs = h // 2, h % 2
                    if is_m:
                        p.op("dve", lambda e, h=h, pr=pr, hs=hs: e.scalar_tensor_tensor(
                            out=hg[:, h, :], in0=numP[pr][:, hs, :], scalar=scl[:, h:h + 1],
                            in1=og[par][:, h * DV:(h + 1) * DV], op0=ALU.mult, op1=ALU.mult),
                            reads=[numK[pr], ("tiny",), ("og", par, h // 2)], writes=[("hg", h)])
                        p.op("act", lambda e, h=h: e.activation(out=junk[:], in_=hg[:, h, :], func=AF.Square,
                                                                accum_out=ss4[:, h:h + 1]),
                             reads=[("hg", h)], writes=[("junk",), tk2])
                    else:
                        p.op("act", lambda e, h=h, pr=pr, hs=hs: e.activation(out=junk[:], in_=numP[pr][:, hs, :], func=AF.Square,
                                                                              accum_out=ss4[:, h:h + 1]),
                             reads=[numK[pr]], writes=[("junk",), tk2])
                p.op("act", lambda e: e.activation(out=sd4, in_=ss4, func=AF.Sqrt, scale=1.0 / DV, bias=EPS),
                     reads=[tk2], writes=[tk2])
                p.op("dve", lambda e: e.reciprocal(out=rs4, in_=sd4), reads=[tk2], writes=[tk2])
                for h in range(H):
                    pr, hs = h // 2, h % 2
                    if is_m:
                        p.op("dve", lambda e, h=h: e.scalar_tensor_tensor(
                            out=hn[:, h, :], in0=hg[:, h, :], scalar=rs4[:, h:h + 1], in1=ong_b[:, h * DV:(h + 1) * DV],
                            op0=ALU.mult, op1=ALU.mult),
                            reads=[("hg", h), tk2, ("ong_b",)], writes=[("hn", h // 2)])
                    else:
                        p.op("dve", lambda e, h=h, pr=pr, hs=hs: e.scalar_tensor_tensor(
                            out=hn[:, h, :], in0=numP[pr][:, hs, :], scalar=rs4[:, h:h + 1],
                            in1=og[par][:, h * DV:(h + 1) * DV], op0=ALU.mult, op1=ALU.mult),
                            reads=[numK[pr], tk2, ("og", par, h // 2)], writes=[("hn", h // 2)])
                def s6():
                    for i in range(8):
                        h, e2 = i // 2, i % 2
                        p.op("pe", lambda e, h=h, e2=e2, i=i: e.transpose(
                            tpS[:, i, :], in_=hn[:, h, e2 * 128:(e2 + 1) * 128], identity=ident[:]),
                            reads=[("hn", h // 2), ("ident",)], writes=[("sc",)], signal=(i == 7))
                    p.op("act", lambda e: e.activation(out=hnT[gp][:, :, cs], in_=tpS[:, :, :], func=AF.Copy),
                         reads=[("sc",)], writes=[("hnT", gp, 0, c), ("hnT", gp, 1, c)])
                state["s6"] = s6
                pend[0:0] = after_s3
                inter(len(pend))
                state["first"] = False

            for st_ in stats_steps(0) + preamble_steps(0) + proj_steps(0, 0):
                st_()
            pend = []
            for g in range(NG):
                for c in range(4):
                    after_s3 = []
                    if c < 3:
                        pend += proj_steps(g, c + 1)
                        if c == 2 and g + 1 < NG:
                            pend += stats_steps(g + 1)
                    else:
                        pre = (preamble_steps(g + 1) + proj_steps(g + 1, 0)) if g + 1 < NG else []
                        opj = outproj_steps(g - 1) if g > 0 else []
                        pend += opj[:4]
                        opj = opj[4:]
                        while pre or opj:
                            if opj:
                                after_s3.append(opj.pop(0))
                            if pre:
                                after_s3.append(pre.pop(0))
                    core_chunk(g, c, pend, after_s3)
                    while pend:
                        pend.pop(0)()
            state["s6"]()
            state["s6"] = None
            for st_ in outproj_steps(NG - 1):
                st_()
            p.barrier()

    def ffn(layer):
        gcol = 16 + layer * 8
        with ExitStack() as ph:
            xn = sb(ph, "xnf", [128, KC, T], BF16)
            hid = sb(ph, "hid", [128, 16, T], BF16)
            w1b = [sb(ph, "w1b", [128, KC, 512], BF16) for _ in range(2)]
            w2b = [sb(ph, "w2b", [128, 16, 256], BF16) for _ in range(2)]
            sq = sb(ph, "sq", [128, 2, GS], BF16)
            sd = sb(ph, "sd", [128, GS], F32)
            rstd = sb(ph, "rstd", [128, GS], F32)
            rt = sb(ph, "rt", [128, 2, GS], F32)
            if dbg == 'mem':
                print('SBUF free in ffn phase', nc.sbuf_bytes_remaining)
            w1src = w1_d[layer].rearrange("(k p) n -> p k n", p=128)
            loads = []
            for hh in range(2):
                for mb in range(4):
                    loads.append(("w1", hh, mb))
                for jb in range(4):
                    loads.append(("w2", hh, jb))
            slot_of = {}
            cnt = dict(w1=0, w2=0)

            def issue(ld):
                kind_, hh, b = ld
                if kind_ == "w1":
                    sl = cnt["w1"] % 2
                    cnt["w1"] += 1
                    col0 = hh * 2048 + b * 512
                    p.dma("pool", "w1b%d" % sl, w1b[sl][:], w1src[:, :, col0:col0 + 512], writes=[("w1b", sl)])
                else:
                    sl = cnt["w2"] % 2
                    cnt["w2"] += 1
                    src = w2_d[layer, hh * 2048:(hh + 1) * 2048, :].rearrange("(m p) n -> p m n", p=128)
                    p.dma("pool", "w2b%d" % sl, w2b[sl][:], src[:, :, b * 256:(b + 1) * 256], writes=[("w2b", sl)])
                slot_of[ld] = sl

            li = 0
            issue(loads[0])
            issue(loads[1])
            li = 2
            for tg in range(NG):
                norm_stats(tg, sq, sd, rstd)
                for k in range(KC):
                    p.op("dve", lambda e, k=k, tg=tg: e.scalar_tensor_tensor(
                        out=xn[:, k, tgs(tg)], in0=xT[:, k, tgs(tg)], scalar=gains[:, gcol + k:gcol + k + 1],
                        in1=rstd[:], op0=ALU.mult, op1=ALU.mult),
                        reads=[("xT", k, tg), ("gains",), ("rstd",)], writes=[("xnf", k, tg)])
            for bi, ld in enumerate(loads):
                kind_, hh, b = ld
                sl = slot_of[ld]
                if kind_ == "w1":
                    for tg in range(NG):
                        for mi in range(4):
                            m = b * 4 + mi
                            a = nxt("acc", 4)
                            for k in range(KC):
                                p.op("pe", lambda e, k=k, tg=tg, a=a, mi=mi, sl=sl: e.matmul(
                                    acc[a][:], lhsT=w1b[sl][:, k, mi * 128:(mi + 1) * 128], rhs=xn[:, k, tgs(tg)],
                                    start=(k == 0), stop=(k == KC - 1)),
                                    reads=[("w1b", sl), ("xnf", k, tg)], writes=[("acc", a)], signal=(k == KC - 1))
                            r2 = nxt("rt", 2)
                            p.op("act", lambda e, a=a, r2=r2: e.activation(out=rt[:, r2, :], in_=acc[a][:], func=AF.Relu),
                                 reads=[("acc", a)], writes=[("rt", r2)])
                            p.op("dve", lambda e, m=m, tg=tg, r2=r2: e.tensor_tensor(
                                out=hid[:, m, tgs(tg)], in0=rt[:, r2, :], in1=rt[:, r2, :], op=ALU.mult),
                                reads=[("rt", r2)], writes=[("hid", m, tg)])
                else:
                    for ji in range(2):
                        j = b * 2 + ji
                        for tg in range(NG):
                            a = nxt("acc", 4)
                            for m in range(16):
                                p.op("pe", lambda e, m=m, tg=tg, a=a, ji=ji, sl=sl: e.matmul(
                                    acc[a][:], lhsT=w2b[sl][:, m, ji * 128:(ji + 1) * 128], rhs=hid[:, m, tgs(tg)],
                                    start=(m == 0), stop=(m == 15)),
                                    reads=[("w2b", sl), ("hid", m, tg)], writes=[("acc", a)], signal=(m == 15))
                            p.op("dve", lambda e, j=j, tg=tg, a=a: e.tensor_tensor(
                                out=xT[:, j, tgs(tg)], in0=acc[a][:], in1=xT[:, j, tgs(tg)], op=ALU.add),
                                reads=[("acc", a), ("xT", j, tg)], writes=[("xT", j, tg)])
                if li < len(loads):
                    issue(loads[li])
                    li += 1
            p.barrier()

    stages = [("mix0", lambda: mixer(0, "mlstm")), ("ffn0", lambda: ffn(0)),
              ("mix1", lambda: mixer(1, "gla")), ("ffn1", lambda: ffn(1))]
    for sname, fn in stages:
        fn()
        if dbg == sname:
            return dump_and_finish()

    with ExitStack() as ph:
        sq = sb(ph, "sq", [128, 2, GS], BF16)
        sd = sb(ph, "sd", [128, GS], F32)
        rstd = sb(ph, "rstd", [128, GS], F32)
        oT = sb(ph, "oT", [128, 2, KC, GS], F32)
        out_v = out_d.rearrange("(k p) t -> p k t", p=128)
        for tg in range(NG):
            norm_stats(tg, sq, sd, rstd)
            o2 = tg % 2
            for k in range(KC):
                p.op("dve", lambda e, k=k, tg=tg, o2=o2: e.scalar_tensor_tensor(
                    out=oT[:, o2, k, :], in0=xT[:, k, tgs(tg)], scalar=gains[:, 32 + k:33 + k],
                    in1=rstd[:], op0=ALU.mult, op1=ALU.mult),
                    reads=[("xT", k, tg), ("gains",), ("rstd",)], writes=[("oT", o2)])
            p.dma("sp", "out%d" % o2, out_v[:, :, tgs(tg)], oT[:, o2, :, :], reads=[("oT", o2)])
        for o2 in range(2):
            d = p.dsem["out%d" % o2]
            nc.sync.wait_ge(d[0], d[1])
    es.close()
    return nc


def prep_inputs(inp):
    f = lambda a: np.ascontiguousarray(np.asarray(a, dtype=np.float32))
    nm = f(inp["norm_mix_g"]).reshape(2, 8, 128).transpose(2, 0, 1).reshape(128, 16)
    nf = f(inp["norm_ffn_g"]).reshape(2, 8, 128).transpose(2, 0, 1).reshape(128, 16)
    fg = f(inp["final_norm_g"]).reshape(8, 128).T
    gains = f(np.concatenate([nm, nf, fg], axis=1))
    shared = {
        "gains": gains,
        "m_win": f(inp["mlstm_w_in"][0]),
        "m_bg": f(inp["mlstm_b_gate"]).reshape(1, 8),
        "m_ong": f(inp["mlstm_out_norm_g"]).reshape(1, 1024),
        "m_wout": f(inp["mlstm_w_out"][0]),
        "g_win": f(inp["gla_w_in"][0]),
        "g_wgu": f(inp["gla_w_gate_up"][0]),
        "g_bg": f(f(inp["gla_b_gate"]).reshape(4, 128).T),
        "g_ong": f(inp["gla_out_norm_g"]).reshape(1, 1024),
        "g_wout": f(inp["gla_w_out"][0]),
        "w1": f(inp["ffn_w1"]),
        "w2": f(inp["ffn_w2"]),
    }
    x = np.asarray(inp["x"], dtype=np.float32)
    in_maps = []
    for b in range(8):
        m = dict(shared)
        m["xT"] = np.ascontiguousarray(x[b].T)
        in_maps.append(m)
    return in_maps


def kernel(**inputs):
    in_maps = prep_inputs(inputs)
    nc = build()
    res = run_bass_kernel_spmd(nc, in_maps, core_ids=list(range(8)))
    out = np.stack([np.ascontiguousarray(res.results[b]["outT"].T) for b in range(8)], axis=0)
    return out.astype(np.float32)
```

```python
import numpy as np
from contextlib import ExitStack
import concourse.bass as bass
import concourse.mybir as mybir
from concourse.bass_utils import run_bass_kernel_spmd

F32 = mybir.dt.float32
BF16 = mybir.dt.bfloat16
AF = mybir.ActivationFunctionType
ALU = mybir.AluOpType

T = 2048
D = 1024
KC = 8
NG = 4
GS = 512
CH = 128
H = 4
DK = 128
DV = 256
DFF = 4096
EPS = 1e-6
QSCALE = DK ** -0.5


class P:
    def __init__(s, nc, es):
        s.nc = nc
        s.es = es
        s.E = {}
        for name, eng in (("pe", nc.tensor), ("act", nc.scalar), ("dve", nc.vector),
                          ("pool", nc.gpsimd), ("sp", nc.sync)):
            s.E[name] = dict(eng=eng, sem=es.enter_context(nc.semaphore("s_" + name)), count=0, waited={})
        s.lastw = {}
        s.readers = {}
        s.dsem = {}
        s.uid = 0

    def name(s, base):
        s.uid += 1
        return "%s_%d" % (base, s.uid)

    def _waits(s, ename, reads, writes):
        e = s.E[ename]
        pesem = s.E["pe"]["sem"]
        need = {}

        def add(sem, val):
            if ename == "pe" and sem is pesem:
                return
            if need.get(sem, 0) < val:
                need[sem] = val
        for k in reads:
            t = s.lastw.get(k)
            if t:
                add(*t)
        for k in writes:
            t = s.lastw.get(k)
            if t:
                add(*t)
            for sem, val in s.readers.get(k, {}).items():
                add(sem, val)
        for sem, val in need.items():
            if e["waited"].get(sem, 0) < val:
                e["eng"].wait_ge(sem, val)
                e["waited"][sem] = val

    def _reg(s, tok, reads, writes):
        for k in reads:
            d = s.readers.setdefault(k, {})
            if d.get(tok[0], 0) < tok[1]:
                d[tok[0]] = tok[1]
        for k in writes:
            s.lastw[k] = tok
            s.readers[k] = {}

    @staticmethod
    def _split(reads, writes):
        def nk(k):
            if k[0] in ("sc", "num", "dC"):
                return (k[0],), True
            if k[0] in ("tp", "small"):
                return ("misc",), True
            if k[0] == "acc":
                return k, True
            return k, False
        r2, w2 = [], []
        for k in reads:
            k2, ex = nk(k)
            (w2 if ex else r2).append(k2)
        for k in writes:
            w2.append(nk(k)[0])
        return r2, w2

    def op(s, ename, fn, reads=(), writes=(), signal=True):
        reads, writes = s._split(reads, writes)
        s._waits(ename, reads, writes)
        e = s.E[ename]
        ins = fn(e["eng"])
        if signal:
            e["count"] += 1
            ins.then_inc(e["sem"], 1)
            tok = (e["sem"], e["count"])
        else:
            tok = (e["sem"], e["count"] + 1)
        s._reg(tok, reads, writes)

    def dma(s, qname, dname, out, in_=None, reads=(), writes=()):
        pairs = out if in_ is None else [(out, in_)]
        s._waits(qname, reads, writes)
        if dname not in s.dsem:
            s.dsem[dname] = [s.es.enter_context(s.nc.semaphore("d_" + dname)), 0]
        d = s.dsem[dname]
        for o, i in pairs:
            d[1] += 16
            s.E[qname]["eng"].dma_start(out=o, in_=i).then_inc(d[0], 16)
        s._reg((d[0], d[1]), reads, writes)
        return (d[0], d[1])

    def barrier(s):
        toks = [(e["sem"], e["count"]) for e in s.E.values() if e["count"] > 0]
        toks += [(d[0], d[1]) for d in s.dsem.values()]
        pesem = s.E["pe"]["sem"]
        for name, e in s.E.items():
            for sem, val in toks:
                if name == "pe" and sem is pesem:
                    continue
                if e["waited"].get(sem, 0) < val:
                    e["eng"].wait_ge(sem, val)
                    e["waited"][sem] = val


def build(dbg=None):
    nc = bass.Bass("TRN2", target_bir_lowering=False)

    def dram(name, shape, kind="ExternalInput"):
        return nc.dram_tensor(name, list(shape), F32, kind=kind).ap()

    xT_d = dram("xT", [D, T])
    gains_d = dram("gains", [128, 40])
    m_win_d = dram("m_win", [D, 3080])
    m_bg_d = dram("m_bg", [1, 8])
    m_ong_d = dram("m_ong", [1, 1024])
    m_wout_d = dram("m_wout", [D, D])
    g_win_d = dram("g_win", [D, 3088])
    g_wgu_d = dram("g_wgu", [16, 512])
    g_bg_d = dram("g_bg", [128, 4])
    g_ong_d = dram("g_ong", [1, 1024])
    g_wout_d = dram("g_wout", [D, D])
    w1_d = dram("w1", [2, D, DFF])
    w2_d = dram("w2", [2, DFF, D])
    out_d = dram("outT", [D, T], kind="ExternalOutput")

    es = ExitStack()
    p = P(nc, es)

    def sb(stack, base, shape, dt):
        return stack.enter_context(nc.sbuf_tensor(p.name(base), list(shape), dt))

    def ps(base, shape, dt):
        return es.enter_context(nc.psum_tensor(p.name(base), list(shape), dt))

    xT = sb(es, "xT", [128, KC, T], F32)
    gains = sb(es, "gains", [128, 40], F32)
    ident = sb(es, "ident", [128, 128], BF16)
    tri_f = sb(es, "tri_f", [128, 128], F32)
    ones_f = sb(es, "ones_f", [128, 128], F32)
    ones_bf = sb(es, "ones_bf", [128, 128], BF16)
    rmask = sb(es, "rmask", [128, GS], F32)

    acc = [ps("acc", [128, 512], F32) for _ in range(4)]
    sc = ps("sc", [128, 4, 128], F32)
    num = ps("num", [128, 2, 256], F32)
    dC = ps("dC", [128, 2, 256], F32)
    tpsm = ps("tpsm", [128, 512], F32)
    tp = tpsm[:, 0:256].bitcast(BF16).rearrange("p (s n) -> p s n", n=128)
    small = tpsm[:, 256:512]
    rr = dict(accm=0, acc=0, sc=0, num=0, dC=0, tp=0, den=0, rt=0)

    def nxt(nm, n):
        i = rr[nm] % n
        rr[nm] = (i + 1) % n
        return i

    def emit_consts():
        p.op("dve", lambda e: e.memset(ident[:], 0.0), writes=[("ident",)])
        p.op("dve", lambda e: e.memset(ones_f[:], 1.0), writes=[("ones_f",)])
        p.op("dve", lambda e: e.memset(ones_bf[:], 1.0), writes=[("ones_bf",)])
        p.op("dve", lambda e: e.memset(tri_f[:], 1.0), writes=[("tri_f",)])
        p.op("dve", lambda e: e.memset(rmask[:], 1.0), writes=[("rmask",)])
        p.op("dve", lambda e: e.memset(rmask[:].rearrange("p (c t) -> p c t", t=CH)[:, :, 0:1], 0.0),
             reads=[("rmask",)], writes=[("rmask",)])
        p.op("pool", lambda e: e.affine_select(out=ident[:], in_=ident[:], compare_op=ALU.not_equal, fill=1.0,
                                               base=0, pattern=[[-1, 128]], channel_multiplier=1),
             reads=[("ident",)], writes=[("ident",)])
        p.op("pool", lambda e: e.affine_select(out=tri_f[:], in_=tri_f[:], compare_op=ALU.is_ge, fill=0.0,
                                               base=0, pattern=[[1, 128]], channel_multiplier=-1),
             reads=[("tri_f",)], writes=[("tri_f",)])

    p.dma("sp", "gains", gains[:], gains_d, writes=[("gains",)])
    xT_v = xT_d.rearrange("(k p) t -> p k t", p=128)
    def load_xT(q, tg):
        p.dma(q, "xT%d" % tg, [(xT[:, k0:k0 + 4, tg * GS:(tg + 1) * GS], xT_v[:, k0:k0 + 4, tg * GS:(tg + 1) * GS])
                               for k0 in (0, 4)],
              writes=[("xT", k, tg) for k in range(KC)])
    load_xT("sp", 0)

    def tgs(tg):
        return slice(tg * GS, (tg + 1) * GS)

    def norm_stats(tg, sq, sd, rstd, banks=None, sdk=("sd",), rk=("rstd",)):
        sd = sd if hasattr(sd, "tensor") else sd[:]
        rstd = rstd if hasattr(rstd, "tensor") else rstd[:]
        a = nxt("acc", 4) if banks is None else banks[nxt("accm", len(banks))]
        for k in range(KC):
            sl = k % 2
            p.op("act", lambda e, k=k, sl=sl: e.activation(out=sq[:, sl, :], in_=xT[:, k, tgs(tg)], func=AF.Square),
                 reads=[("xT", k, tg)], writes=[("sq", sl)])
            p.op("pe", lambda e, k=k, sl=sl: e.matmul(acc[a][:], lhsT=ones_bf[:], rhs=sq[:, sl, :],
                                                     start=(k == 0), stop=(k == KC - 1)),
                 reads=[("sq", sl), ("ones_bf",)], writes=[("acc", a)], signal=True)
        p.op("act", lambda e: e.activation(out=sd, in_=acc[a][:], func=AF.Ln, scale=1.0 / D, bias=EPS),
             reads=[("acc", a)], writes=[sdk])
        p.op("act", lambda e: e.activation(out=rstd, in_=sd, func=AF.Exp, scale=-0.5), reads=[sdk], writes=[rk])

    def dump_and_finish():
        p.dma("sp", "out", [(out_d[k * 128:(k + 1) * 128, :], xT[:, k, :]) for k in range(KC)],
              reads=[("xT", k, tg) for k in range(KC) for tg in range(NG)])
        d = p.dsem["out"]
        nc.sync.wait_ge(d[0], d[1])
        es.close()
        return nc

    def mixer(layer, kind):
        is_m = (kind == "mlstm")
        win_d = m_win_d if is_m else g_win_d
        wout_d = m_wout_d if is_m else g_wout_d
        ong_d = m_ong_d if is_m else g_ong_d
        NIN = 3080 if is_m else 3088
        gcol = layer * 8
        numP = [num, acc[2][:].rearrange("p (s n) -> p s n", n=DV)]
        numK = [("num",), ("acc", 2)]
        dCP = [dC, dC]
        dCK = [("dC",), ("dC",)]
        PB = [0, 1, 3]
        tpS = sc[:].rearrange("p s n -> p (s n)").bitcast(BF16).rearrange("p (s n) -> p s n", n=128)
        tpK = dC[:].rearrange("p s n -> p (s n)").bitcast(BF16).rearrange("p (s n) -> p s n", n=128)
        with ExitStack() as ph:
            win = sb(ph, "win", [128, KC, NIN], BF16)
            wout = sb(ph, "wout", [128, KC, D], BF16)
            ong_b = sb(ph, "ong_b", [128, 1024], F32)
            xn = sb(ph, "xn", [128, KC, GS], BF16)
            sq = sb(ph, "sq", [128, 2, GS], BF16)
            sd = sb(ph, "sd", [128, GS], F32)
            rstd = sb(ph, "rstd", [128, GS], F32)
            qT = sb(ph, "qT", [128, H, GS], BF16)
            kT = sb(ph, "kT", [128, H, GS], BF16)
            ktok = [sb(ph, "ktok", [128, H, DK], BF16) for _ in range(2)]
            vt = [sb(ph, "vt", [128, H, DV], BF16) for _ in range(2)]
            og = [sb(ph, "og", [128, H * DV], F32) for _ in range(2)]
            St = sb(ph, "St", [128, H, 128], BF16)
            tri4 = sb(ph, "tri4", [128, H, 128], BF16)
            junk = sb(ph, "junk", [128, DV], BF16)
            hn = sb(ph, "hn", [128, H, DV], BF16)
            hnT = [sb(ph, "hnT", [128, KC, GS], BF16) for _ in range(2)]
            R32 = sb(ph, "R32", [128, H, DV], F32)
            Cbf = sb(ph, "Cbf", [128, H, DV], BF16)
            sm = sb(ph, "sm", [128, 16, 16], F32)
            egs = sb(ph, "egs", [128, 2, 16], F32)
            tiny = sb(ph, "tiny", [128, 16, 4], F32)
            if is_m:
                hg = sb(ph, "hg", [128, H, DV], F32)
                n32 = sb(ph, "n32", [128, H], F32)
                nbf = sb(ph, "nbf", [128, H], BF16)
                ubf = sb(ph, "ubf", [128, 2, 16], BF16)
                bg4 = sb(ph, "bg4", [128, 4, 8], F32)
                gates = sb(ph, "gates", [128, 4, 8], F32)
            else:
                wgu = sb(ph, "wgu", [16, 512], F32)
                gbg = sb(ph, "gbg", [128, 4], F32)
                ngbg = sb(ph, "ngbg", [128, 4], F32)
                zl = sq[0:16, :, :].rearrange("p a t -> p (a t)").bitcast(F32)
                e1 = sd[:].rearrange("p (o t) -> p o t", o=1)
                nbT = rstd[:].rearrange("p (o t) -> p o t", o=1)
                EB = sb(ph, "EB", [128, 1, GS], F32)
                EK = sb(ph, "EK", [128, 1, GS], F32)

            if dbg == 'mem':
                print('SBUF free in mixer phase', kind, nc.sbuf_bytes_remaining)
            win_src = win_d.rearrange("(k p) n -> p k n", p=128)
            p.dma("pool", "winZ", [(win[:, :, 3072:NIN], win_src[:, :, 3072:NIN])], writes=[("win", "gate")])
            p.dma("pool", "winA", [(win[:, k0:k0 + 4, 0:1024], win_src[:, k0:k0 + 4, 0:1024]) for k0 in range(0, KC, 4)],
                  writes=[("win", "qk")])
            p.dma("pool", "winB", [(win[:, k0:k0 + 2, 1024:3072], win_src[:, k0:k0 + 2, 1024:3072]) for k0 in range(0, KC, 2)],
                  writes=[("win", "rest")])
            if layer == 0:
                for tg in range(1, NG):
                    load_xT("pool", tg)
                emit_consts()
            wout_src = wout_d.rearrange("(k p) n -> p k n", p=128)
            p.dma("pool", "wout", [(wout[:, k0:k0 + 4, :], wout_src[:, k0:k0 + 4, :]) for k0 in range(0, KC, 4)],
                  writes=[("wout",)])
            if is_m:
                p.dma("sp", "misc", [(ong_b[:], ong_d.partition_broadcast(128))] +
                      [(bg4[:, c, :], m_bg_d.partition_broadcast(128)) for c in range(4)],
                      writes=[("ong_b",), ("bg4",)])
            else:
                p.dma("sp", "misc", [(ong_b[:], ong_d.partition_broadcast(128)), (wgu[:], g_wgu_d), (gbg[:], g_bg_d)],
                      writes=[("ong_b",), ("wgu",), ("gbg",)])
                p.op("dve", lambda e: e.tensor_scalar(out=ngbg[:], in0=gbg[:], scalar1=-1.0, scalar2=None,
                                                      op0=ALU.mult), reads=[("gbg",)], writes=[("ngbg",)])
            for h in range(H):
                p.op("pool", lambda e, h=h: e.tensor_copy(out=tri4[:, h, :], in_=tri_f[:]), reads=[("tri_f",)],
                     writes=[("tri4",)])

            state = dict(first=True)

            def proj_fm(col0, M, a):
                for k in range(KC):
                    p.op("pe", lambda e, k=k: e.matmul(acc[a][0:M, :], lhsT=win[:, k, col0:col0 + M],
                                                      rhs=xn[:, k, :], start=(k == 0), stop=(k == KC - 1)),
                         reads=[("win", "qk" if col0 < 1024 else ("gate" if col0 >= 3072 else "rest")), ("xn", k)], writes=[("acc", a)], signal=(k == KC - 1))

            def proj_tm(c, col0, N, a):
                for k in range(KC):
                    p.op("pe", lambda e, k=k: e.matmul(acc[a][:, 0:N], lhsT=xn[:, k, c * CH:(c + 1) * CH],
                                                      rhs=win[:, k, col0:col0 + N],
                                                      start=(k == 0), stop=(k == KC - 1)),
                         reads=[("win", "rest"), ("xn", k)], writes=[("acc", a)], signal=(k == KC - 1))

            c4 = lambda ap: ap.rearrange("p (c h) -> p c h", h=4)

            def stats_steps(g):
                return [lambda: norm_stats(g, sq, sd, rstd, banks=PB)]

            def preamble_steps(g):
                gp = g % 2
                steps = []

                def s_xn():
                    for k in range(KC):
                        p.op("dve", lambda e, k=k: e.scalar_tensor_tensor(
                            out=xn[:, k, :], in0=xT[:, k, tgs(g)], scalar=gains[:, gcol + k:gcol + k + 1],
                            in1=rstd[:], op0=ALU.mult, op1=ALU.mult),
                            reads=[("xT", k, g), ("gains",), ("rstd",)], writes=[("xn", k)])
                steps.append(s_xn)

                def s_q(h):
                    a = PB[nxt("accm", 3)]
                    proj_fm(h * DK, DK, a)
                    if is_m:
                        p.op("act", lambda e: e.activation(out=qT[:, h, :], in_=acc[a][:], func=AF.Copy),
                             reads=[("acc", a)], writes=[("qT", h)])
                    else:
                        p.op("dve", lambda e: e.scalar_tensor_tensor(
                            out=qT[:, h, :], in0=acc[a][:], scalar=QSCALE, in1=EB[:, 0, :], op0=ALU.mult, op1=ALU.mult),
                            reads=[("acc", a), ("EB",)], writes=[("qT", h)])

                def s_k(h):
                    a = PB[nxt("accm", 3)]
                    proj_fm(512 + h * DK, DK, a)
                    if is_m:
                        p.op("act", lambda e: e.activation(out=kT[:, h, :], in_=acc[a][:], func=AF.Copy, scale=QSCALE),
                             reads=[("acc", a)], writes=[("kT", h)])
                    else:
                        p.op("dve", lambda e: e.tensor_tensor(
                            out=kT[:, h, :], in0=acc[a][:], in1=EK[:, 0, :], op=ALU.mult),
                            reads=[("acc", a), ("EK",)], writes=[("kT", h)])

                if is_m:
                    ef, spt, tmpu = sm[:, 0, :], sm[:, 1, :], sm[:, 2, :]
                    u, eb = sm[:, 8 + gp, :], sm[:, 10 + gp, :]

                    def s_gates_a():
                        for c in range(4):
                            for k in range(KC):
                                p.op("pe", lambda e, k=k, c=c: e.matmul(
                                    small[:, c * 8:(c + 1) * 8], lhsT=xn[:, k, c * CH:(c + 1) * CH],
                                    rhs=win[:, k, 3072:3080], start=(k == 0), stop=(k == KC - 1)),
                                    reads=[("win", "gate"), ("xn", k)], writes=[("small", "gates")], signal=(k == KC - 1))
                        p.op("dve", lambda e: e.tensor_tensor(
                            out=gates[:], in0=small[:, 0:32].rearrange("p (c n) -> p c n", n=8), in1=bg4[:], op=ALU.add),
                            reads=[("small", "gates"), ("bg4",)], writes=[("gates",)])
                        p.op("act", lambda e: e.activation(out=c4(ef), in_=gates[:, :, 4:8], func=AF.Exp, scale=-1.0),
                             reads=[("gates",)], writes=[("sm", 0)])
                        p.op("act", lambda e: e.activation(out=spt, in_=ef, func=AF.Ln, bias=1.0),
                             reads=[("sm", 0)], writes=[("sm", 1)])

                    def s_gates_b():
                        p.op("pe", lambda e: e.matmul(small[:, 32:48], lhsT=tri_f[:], rhs=spt, start=True, stop=True),
                             reads=[("tri_f",), ("sm", 1)], writes=[("small", "bs")])
                        p.op("pe", lambda e: e.matmul(small[:, 48:64], lhsT=ones_f[:], rhs=spt, start=True, stop=True),
                             reads=[("ones_f",), ("sm", 1)], writes=[("small", "gs")])
                        p.op("dve", lambda e: e.tensor_tensor(out=c4(tmpu), in0=c4(small[:, 32:48]), in1=gates[:, :, 0:4],
                                                              op=ALU.add),
                             reads=[("small", "bs"), ("gates",)], writes=[("sm", 2)])
                        p.op("act", lambda e: e.activation(out=u, in_=tmpu, func=AF.Exp), reads=[("sm", 2)],
                             writes=[("sm", 8 + gp)])
                        p.op("act", lambda e: e.activation(out=eb, in_=small[:, 32:48], func=AF.Exp),
                             reads=[("small", "bs")], writes=[("sm", 10 + gp)])
                        p.op("act", lambda e: e.activation(out=egs[:, gp, :], in_=small[:, 48:64], func=AF.Exp, scale=-1.0),
                             reads=[("small", "gs")], writes=[("egs", gp)])
                        p.op("dve", lambda e: e.tensor_copy(out=ubf[:, gp, :], in_=u), reads=[("sm", 8 + gp)],
                             writes=[("ubf", gp)])
                    steps += [s_gates_a, lambda: s_q(0), lambda: s_k(0), s_gates_b]
                    for h in range(1, H):
                        steps += [lambda h=h: s_q(h), lambda h=h: s_k(h)]
                else:
                    def s_zl():
                        a = PB[nxt("accm", 3)]
                        proj_fm(3072, 16, a)
                        p.op("dve", lambda e: e.tensor_copy(out=zl, in_=acc[a][0:16, :]),
                             reads=[("acc", a)], writes=[("sq", 0), ("sq", 1)])

                    def s_z1(h):
                        a = PB[nxt("accm", 3)]
                        p.op("pe", lambda e: e.matmul(acc[a][:], lhsT=wgu[:, h * DK:(h + 1) * DK], rhs=zl,
                                                      start=True, stop=True),
                             reads=[("wgu",), ("sq", 0), ("sq", 1)], writes=[("acc", a)])
                        p.op("act", lambda e: e.activation(out=e1[:, 0, :], in_=acc[a][:], func=AF.Exp,
                                                           scale=-1.0, bias=ngbg[:, h:h + 1]),
                             reads=[("acc", a), ("ngbg",)], writes=[("sd",)])
                        p.op("act", lambda e: e.activation(out=e1[:, 0, :], in_=e1[:, 0, :], func=AF.Ln, bias=1.0),
                             reads=[("sd",)], writes=[("sd",)])
                        p.op("dve", lambda e: e.tensor_tensor_scan(out=nbT[:, 0, :], data0=rmask[:], data1=e1[:, 0, :],
                                                                  initial=0.0, op0=ALU.mult, op1=ALU.add),
                             reads=[("rmask",), ("sd",)], writes=[("rstd",)])

                    def s_z2(h):
                        p.op("act", lambda e: e.activation(out=EB[:, 0, :], in_=nbT[:, 0, :], func=AF.Exp,
                                                           scale=-1.0 / 16.0),
                             reads=[("rstd",)], writes=[("EB",)])
                        p.op("act", lambda e: e.activation(out=EK[:, 0, :], in_=nbT[:, 0, :], func=AF.Exp, scale=1.0 / 16.0),
                             reads=[("rstd",)], writes=[("EK",)])
                        p.op("dve", lambda e: e.tensor_copy(
                            out=egs[:, gp, h * 4:(h + 1) * 4],
                            in_=EB[:, 0, :].rearrange("p (c t) -> p c t", t=CH)[:, :, CH - 1]),
                            reads=[("EB",)], writes=[("egs", gp)])

                    def s_head(h):
                        a1 = PB[nxt("accm", 3)]
                        proj_fm(h * DK, DK, a1)
                        a2 = PB[nxt("accm", 3)]
                        proj_fm(512 + h * DK, DK, a2)
                        p.op("dve", lambda e: e.scalar_tensor_tensor(
                            out=qT[:, h, :], in0=acc[a1][:], scalar=QSCALE, in1=EB[:, 0, :], op0=ALU.mult, op1=ALU.mult),
                            reads=[("acc", a1), ("EB",)], writes=[("qT", h)])
                        p.op("dve", lambda e: e.tensor_tensor(
                            out=kT[:, h, :], in0=acc[a2][:], in1=EK[:, 0, :], op=ALU.mult),
                            reads=[("acc", a2), ("EK",)], writes=[("kT", h)])
                        if h + 1 < H:
                            s_z1(h + 1)
                            s_z2(h + 1)
                    steps += [s_zl, lambda: (s_z1(0), s_z2(0))]
                    for h in range(H):
                        steps.append(lambda h=h: s_head(h))
                return steps

            def proj_steps(g, c):
                par = c % 2
                gp = g % 2
                steps = []
                for vb in range(2):
                    def s_v(vb=vb):
                        a = PB[nxt("accm", 3)]
                        proj_tm(c, 1024 + vb * 512, 512, a)
                        if is_m:
                            for hh in range(2):
                                h = vb * 2 + hh
                                p.op("act", lambda e, h=h, hh=hh: e.activation(
                                    out=vt[par][:, h, :], in_=acc[a][:, hh * DV:(hh + 1) * DV], func=AF.Identity,
                                    scale=sm[:, 8 + gp, c * 4 + h:c * 4 + h + 1]),
                                    reads=[("acc", a), ("sm", 8 + gp)], writes=[("vt", par, vb)])
                        else:
                            p.op("act", lambda e: e.activation(
                                out=vt[par][:, vb * 2:vb * 2 + 2, :].rearrange("p h d -> p (h d)"), in_=acc[a][:],
                                func=AF.Copy),
                                reads=[("acc", a)], writes=[("vt", par, vb)])
                    steps.append(s_v)
                for vb in range(2):
                    def s_o(vb=vb):
                        a = PB[nxt("accm", 3)]
                        proj_tm(c, 2048 + vb * 512, 512, a)
                        if is_m:
                            p.op("act", lambda e: e.activation(out=og[par][:, vb * 512:(vb + 1) * 512], in_=acc[a][:],
                                                               func=AF.Sigmoid),
                                 reads=[("acc", a)], writes=[("og", par, vb)])
                        else:
                            p.op("act", lambda e: e.activation(out=og[par][:, vb * 512:(vb + 1) * 512], in_=acc[a][:],
                                                               func=AF.Silu),
                                 reads=[("acc", a)], writes=[("og", par, vb)])
                            p.op("pool", lambda e: e.tensor_tensor(
                                out=og[par][:, vb * 512:(vb + 1) * 512], in0=og[par][:, vb * 512:(vb + 1) * 512],
                                in1=ong_b[:, vb * 512:(vb + 1) * 512], op=ALU.mult),
                                reads=[("og", par, vb), ("ong_b",)], writes=[("og", par, vb)])
                    steps.append(s_o)
                return steps

            def outproj_steps(g):
                gp = g % 2
                steps = []
                for j in range(KC):
                    def s_j(j=j):
                        a = PB[nxt("accm", 3)]
                        for ec in range(KC):
                            p.op("pe", lambda e, ec=ec: e.matmul(
                                acc[a][:], lhsT=wout[:, ec, j * 128:(j + 1) * 128], rhs=hnT[gp][:, ec, :],
                                start=(ec == 0), stop=(ec == KC - 1)),
                                reads=[("wout",)] + [("hnT", gp, ec // 4, c) for c in range(4)], writes=[("acc", a)],
                                signal=(ec == KC - 1))
                        p.op("dve", lambda e: e.tensor_tensor(out=xT[:, j, tgs(g)], in0=acc[a][:], in1=xT[:, j, tgs(g)],
                                                              op=ALU.add),
                             reads=[("acc", a), ("xT", j, g)], writes=[("xT", j, g)])
                    steps.append(s_j)
                return steps

            def core_chunk(g, c, pend, after_s3):
                gp = g % 2
                cs = slice(c * CH, (c + 1) * CH)
                par = c % 2
                first = state["first"]

                def inter(n=1):
                    for _ in range(n):
                        if pend:
                            pend.pop(0)()

                if is_m:
                    egprev = (egs[:, gp, (c - 1) * 4:c * 4] if c > 0 else egs[:, 1 - gp, 12:16])
                    egcur = egs[:, gp, c * 4:(c + 1) * 4]
                else:
                    ev = lambda par_: egs[:, par_, :].rearrange("p (h c) -> p h c", c=4)
                    egprev = (ev(gp)[:, :, c - 1] if c > 0 else ev(1 - gp)[:, :, 3])
                    egcur = ev(gp)[:, :, c]
                egprev_key = ("egs", gp) if c > 0 else ("egs", 1 - gp)
                egcur_key = ("egs", gp)
                for h in range(H):
                    p.op("pe", lambda e, h=h: e.matmul(sc[:, h, :], lhsT=kT[:, h, cs], rhs=qT[:, h, cs],
                                                       start=True, stop=True),
                         reads=[("kT", h), ("qT", h)], writes=[("sc",)], signal=(h == H - 1))
                if True:
                    for h in range(H):
                        p.op("pe", lambda e, h=h: e.transpose(tpK[:, h, :], in_=kT[:, h, cs], identity=ident[:]),
                             reads=[("kT", h), ("ident",)], writes=[("dC",)], signal=(h == H - 1))
                    p.op("act", lambda e: e.activation(out=ktok[par][:], in_=tpK[:, 0:4, :], func=AF.Copy),
                         reads=[("dC",)], writes=[("ktok", par)])
                p.op("dve", lambda e: e.tensor_tensor(out=St[:], in0=sc[:, :, :], in1=tri4[:], op=ALU.mult),
                     reads=[("sc",), ("tri4",)], writes=[("St",)])
                inter(len(pend))
                for h in range(H):
                    pr, hs = h // 2, h % 2
                    p.op("pe", lambda e, h=h, pr=pr, hs=hs: e.matmul(numP[pr][:, hs, :], lhsT=St[:, h, :], rhs=vt[par][:, h, :],
                                                                    start=True, stop=first),
                         reads=[("St",), ("vt", par, h // 2)], writes=[numK[pr]], signal=(first and hs == 1))
                    if not first:
                        p.op("pe", lambda e, h=h, pr=pr, hs=hs: e.matmul(numP[pr][:, hs, :], lhsT=qT[:, h, cs], rhs=Cbf[:, h, :],
                                                                        start=False, stop=True),
                             reads=[("qT", h), ("Cbf",)], writes=[numK[pr]], signal=(hs == 1))
                if is_m:
                    for h in range(H):
                        p.op("pe", lambda e, h=h: e.matmul(
                            small[:, 64 + h:65 + h], lhsT=St[:, h, :], rhs=ubf[:, gp, c * 4 + h:c * 4 + h + 1],
                            start=True, stop=first),
                            reads=[("St",), ("ubf", gp)], writes=[("small", "den")], signal=False)
                        if not first:
                            p.op("pe", lambda e, h=h: e.matmul(
                                small[:, 64 + h:65 + h], lhsT=qT[:, h, cs], rhs=nbf[:, h:h + 1],
                                start=False, stop=True),
                                reads=[("qT", h), ("nbf",)], writes=[("small", "den")], signal=False)
                    for h in range(H):
                        p.op("pe", lambda e, h=h: e.matmul(
                            small[:, 68 + h:69 + h], lhsT=ktok[par][:, h, :], rhs=ubf[:, gp, c * 4 + h:c * 4 + h + 1],
                            start=True, stop=True),
                            reads=[("ktok", par), ("ubf", gp)], writes=[("small", "dn")], signal=(h == H - 1))
                def dc_pair(pr):
                    for hs in range(2):
                        h = pr * 2 + hs
                        p.op("pe", lambda e, h=h, hs=hs: e.matmul(dC[:, hs, :], lhsT=ktok[par][:, h, :], rhs=vt[par][:, h, :],
                                                                  start=True, stop=True),
                             reads=[("ktok", par), ("vt", par, h // 2)], writes=[("dC",)], signal=(hs == 1))
                    for hs in range(2):
                        h = pr * 2 + hs
                        if first:
                            p.op("dve", lambda e, h=h, hs=hs: e.tensor_copy(out=R32[:, h, :], in_=dC[:, hs, :]),
                                 reads=[("dC",)], writes=[("R32",)])
                        else:
                            p.op("dve", lambda e, h=h, hs=hs: e.scalar_tensor_tensor(
                                out=R32[:, h, :], in0=R32[:, h, :], scalar=egprev[:, h:h + 1], in1=dC[:, hs, :],
                                op0=ALU.mult, op1=ALU.add),
                                reads=[("R32",), ("dC",), egprev_key], writes=[("R32",)])
                dc_pair(0)
                if state.get("s6"):
                    state["s6"]()
                    state["s6"] = None
                dc_pair(1)
                if is_m:
                    ebi = sm[:, 10 + gp, c * 4:(c + 1) * 4]
                    tt_ = [tiny[:, i, :] for i in range(8)]
                    tk = ("tiny",)
                    p.op("dve", lambda e: e.tensor_tensor(out=tt_[0], in0=small[:, 64:68], in1=ebi, op=ALU.max),
                         reads=[("small", "den"), ("sm", 10 + gp)], writes=[tk])
                    p.op("dve", lambda e: e.scalar_tensor_tensor(out=tt_[1], in0=small[:, 64:68], scalar=-1.0, in1=tt_[0],
                                                                 op0=ALU.mult, op1=ALU.max),
                         reads=[("small", "den"), tk], writes=[tk])
                    p.op("dve", lambda e: e.reciprocal(out=tt_[5], in_=tt_[1]), reads=[tk], writes=[tk])
                    scl = tt_[5]
                p.op("pool", lambda e: e.tensor_tensor(
                    out=Cbf[:], in0=R32[:], in1=egcur.unsqueeze(2).broadcast_to([128, H, DV]), op=ALU.mult),
                    reads=[("R32",), egcur_key], writes=[("Cbf",)])
                if is_m:
                    if first:
                        p.op("dve", lambda e: e.tensor_copy(out=n32[:], in_=small[:, 68:72]),
                             reads=[("small", "dn")], writes=[("n32",)])
                    else:
                        p.op("dve", lambda e: e.tensor_tensor(out=n32[:], in0=n32[:], in1=egprev, op=ALU.mult),
                             reads=[("n32",), egprev_key], writes=[("n32",)])
                        p.op("dve", lambda e: e.tensor_tensor(out=n32[:], in0=small[:, 68:72], in1=n32[:], op=ALU.add),
                             reads=[("n32",), ("small", "dn")], writes=[("n32",)])
                    p.op("pool", lambda e: e.tensor_tensor(out=nbf[:], in0=n32[:], in1=egcur, op=ALU.mult),
                         reads=[("n32",), egcur_key], writes=[("nbf",)])
                ss4, sd4, rs4 = tiny[:, 8, :], tiny[:, 9, :], tiny[:, 10, :]
                tk2 = ("tiny2",)
                for h in range(H):
                    pr, hs = h // 2, h % 2
                    if is_m:
                        p.op("dve", lambda e, h=h, pr=pr, hs=hs: e.scalar_tensor_tensor(
                            out=hg[:, h, :], in0=numP[pr][:, hs, :], scalar=scl[:, h:h + 1],
                            in1=og[par][:, h * DV:(h + 1) * DV], op0=ALU.mult, op1=ALU.mult),
                            reads=[numK[pr], ("tiny",), ("og", par, h // 2)], writes=[("hg", h)])
                        p.op("act", lambda e, h=h: e.activation(out=junk[:], in_=hg[:, h, :], func=AF.Square,
                                                                accum_out=ss4[:, h:h + 1]),
                             reads=[("hg", h)], writes=[("junk",), tk2])
                    else:
                        p.op("act", lambda e, h=h, pr=pr, hs=hs: e.activation(out=junk[:], in_=numP[pr][:, hs, :], func=AF.Square,
                                                                              accum_out=ss4[:, h:h + 1]),
                             reads=[numK[pr]], writes=[("junk",), tk2])
                p.op("act", lambda e: e.activation(out=sd4, in_=ss4, func=AF.Sqrt, scale=1.0 / DV, bias=EPS),
                     reads=[tk2], writes=[tk2])
                p.op("dve", lambda e: e.reciprocal(out=rs4, in_=sd4), reads=[tk2], writes=[tk2])
                for h in range(H):
                    pr, hs = h // 2, h % 2
                    if is_m:
                        p.op("dve", lambda e, h=h: e.scalar_tensor_tensor(
                            out=hn[:, h, :], in0=hg[:, h, :], scalar=rs4[:, h:h + 1], in1=ong_b[:, h * DV:(h + 1) * DV],
                            op0=ALU.mult, op1=ALU.mult),
                            reads=[("hg", h), tk2, ("ong_b",)], writes=[("hn", h // 2)])
                    else:
                        p.op("dve", lambda e, h=h, pr=pr, hs=hs: e.scalar_tensor_tensor(
                            out=hn[:, h, :], in0=numP[pr][:, hs, :], scalar=rs4[:, h:h + 1],
                            in1=og[par][:, h * DV:(h + 1) * DV], op0=ALU.mult, op1=ALU.mult),
                            reads=[numK[pr], tk2, ("og", par, h // 2)], writes=[("hn", h // 2)])
                def s6():
                    for i in range(8):
                        h, e2 = i // 2, i % 2
                        p.op("pe", lambda e, h=h, e2=e2, i=i: e.transpose(
                            tpS[:, i, :], in_=hn[:, h, e2 * 128:(e2 + 1) * 128], identity=ident[:]),
                            reads=[("hn", h // 2), ("ident",)], writes=[("sc",)], signal=(i == 7))
                    p.op("act", lambda e: e.activation(out=hnT[gp][:, :, cs], in_=tpS[:, :, :], func=AF.Copy),
                         reads=[("sc",)], writes=[("hnT", gp, 0, c), ("hnT", gp, 1, c)])
                state["s6"] = s6
                pend[0:0] = after_s3
                inter(len(pend))
                state["first"] = False

            for st_ in stats_steps(0) + preamble_steps(0) + proj_steps(0, 0):
                st_()
            pend = []
            for g in range(NG):
                for c in range(4):
                    after_s3 = []
                    if c < 3:
                        pend += proj_steps(g, c + 1)
                        if c == 2 and g + 1 < NG:
                            pend += stats_steps(g + 1)
                    else:
                        pre = (preamble_steps(g + 1) + proj_steps(g + 1, 0)) if g + 1 < NG else []
                        opj = outproj_steps(g - 1) if g > 0 else []
                        pend += opj[:4]
                        opj = opj[4:]
                        while pre or opj:
                            if opj:
                                after_s3.append(opj.pop(0))
                            if pre:
                                after_s3.append(pre.pop(0))
                    core_chunk(g, c, pend, after_s3)
                    while pend:
                        pend.pop(0)()
            state["s6"]()
            state["s6"] = None
            for st_ in outproj_steps(NG - 1):
                st_()
            p.barrier()

    def ffn(layer):
        gcol = 16 + layer * 8
        with ExitStack() as ph:
            xn = sb(ph, "xnf", [128, KC, T], BF16)
            hid = sb(ph, "hid", [128, 16, T], BF16)
            w1b = [sb(ph, "w1b", [128, KC, 512], BF16) for _ in range(2)]
            w2b = [sb(ph, "w2b", [128, 16, 256], BF16) for _ in range(2)]
            sq = sb(ph, "sq", [128, 2, GS], BF16)
            sd = sb(ph, "sd", [128, GS], F32)
            rstd = sb(ph, "rstd", [128, GS], F32)
            rt = sb(ph, "rt", [128, 2, GS], F32)
            if dbg == 'mem':
                print('SBUF free in ffn phase', nc.sbuf_bytes_remaining)
            w1src = w1_d[layer].rearrange("(k p) n -> p k n", p=128)
            loads = []
            for hh in range(2):
                for mb in range(4):
                    loads.append(("w1", hh, mb))
                for jb in range(4):
                    loads.append(("w2", hh, jb))
            slot_of = {}
            cnt = dict(w1=0, w2=0)

            def issue(ld):
                kind_, hh, b = ld
                if kind_ == "w1":
                    sl = cnt["w1"] % 2
                    cnt["w1"] += 1
                    col0 = hh * 2048 + b * 512
                    p.dma("pool", "w1b%d" % sl, w1b[sl][:], w1src[:, :, col0:col0 + 512], writes=[("w1b", sl)])
                else:
                    sl = cnt["w2"] % 2
                    cnt["w2"] += 1
                    src = w2_d[layer, hh * 2048:(hh + 1) * 2048, :].rearrange("(m p) n -> p m n", p=128)
                    p.dma("pool", "w2b%d" % sl, w2b[sl][:], src[:, :, b * 256:(b + 1) * 256], writes=[("w2b", sl)])
                slot_of[ld] = sl

            li = 0
            issue(loads[0])
            issue(loads[1])
            li = 2
            for tg in range(NG):
                norm_stats(tg, sq, sd, rstd)
                for k in range(KC):
                    p.op("dve", lambda e, k=k, tg=tg: e.scalar_tensor_tensor(
                        out=xn[:, k, tgs(tg)], in0=xT[:, k, tgs(tg)], scalar=gains[:, gcol + k:gcol + k + 1],
                        in1=rstd[:], op0=ALU.mult, op1=ALU.mult),
                        reads=[("xT", k, tg), ("gains",), ("rstd",)], writes=[("xnf", k, tg)])
            for bi, ld in enumerate(loads):
                kind_, hh, b = ld
                sl = slot_of[ld]
                if kind_ == "w1":
                    for tg in range(NG):
                        for mi in range(4):
                            m = b * 4 + mi
                            a = nxt("acc", 4)
                            for k in range(KC):
                                p.op("pe", lambda e, k=k, tg=tg, a=a, mi=mi, sl=sl: e.matmul(
                                    acc[a][:], lhsT=w1b[sl][:, k, mi * 128:(mi + 1) * 128], rhs=xn[:, k, tgs(tg)],
                                    start=(k == 0), stop=(k == KC - 1)),
                                    reads=[("w1b", sl), ("xnf", k, tg)], writes=[("acc", a)], signal=(k == KC - 1))
                            r2 = nxt("rt", 2)
                            p.op("act", lambda e, a=a, r2=r2: e.activation(out=rt[:, r2, :], in_=acc[a][:], func=AF.Relu),
                                 reads=[("acc", a)], writes=[("rt", r2)])
                            p.op("dve", lambda e, m=m, tg=tg, r2=r2: e.tensor_tensor(
                                out=hid[:, m, tgs(tg)], in0=rt[:, r2, :], in1=rt[:, r2, :], op=ALU.mult),
                                reads=[("rt", r2)], writes=[("hid", m, tg)])
                else:
                    for ji in range(2):
                        j = b * 2 + ji
                        for tg in range(NG):
                            a = nxt("acc", 4)
                            for m in range(16):
                                p.op("pe", lambda e, m=m, tg=tg, a=a, ji=ji, sl=sl: e.matmul(
                                    acc[a][:], lhsT=w2b[sl][:, m, ji * 128:(ji + 1) * 128], rhs=hid[:, m, tgs(tg)],
                                    start=(m == 0), stop=(m == 15)),
                                    reads=[("w2b", sl), ("hid", m, tg)], writes=[("acc", a)], signal=(m == 15))
                            p.op("dve", lambda e, j=j, tg=tg, a=a: e.tensor_tensor(
                                out=xT[:, j, tgs(tg)], in0=acc[a][:], in1=xT[:, j, tgs(tg)], op=ALU.add),
                                reads=[("acc", a), ("xT", j, tg)], writes=[("xT", j, tg)])
                if li < len(loads):
                    issue(loads[li])
                    li += 1
            p.barrier()

    stages = [("mix0", lambda: mixer(0, "mlstm")), ("ffn0", lambda: ffn(0)),
              ("mix1", lambda: mixer(1, "gla")), ("ffn1", lambda: ffn(1))]
    for sname, fn in stages:
        fn()
        if dbg == sname:
            return dump_and_finish()

    with ExitStack() as ph:
        sq = sb(ph, "sq", [128, 2, GS], BF16)
        sd2 = sb(ph, "sd2", [128, 2, GS], F32)
        rstd4 = sb(ph, "rstd4", [128, NG, GS], F32)
        oT = sb(ph, "oT", [128, 2, KC, GS], F32)
        out_v = out_d.rearrange("(k p) t -> p k t", p=128)
        for tg in range(NG):
            norm_stats(tg, sq, sd2[:, tg % 2, :], rstd4[:, tg, :], sdk=("sd", tg % 2), rk=("rstd", tg))
        for tg in range(NG):
            o2 = tg % 2
            for k in range(KC):
                p.op("dve", lambda e, k=k, tg=tg, o2=o2: e.scalar_tensor_tensor(
                    out=oT[:, o2, k, :], in0=xT[:, k, tgs(tg)], scalar=gains[:, 32 + k:33 + k],
                    in1=rstd4[:, tg, :], op0=ALU.mult, op1=ALU.mult),
                    reads=[("xT", k, tg), ("gains",), ("rstd", tg)], writes=[("oT", o2)])
            p.dma("sp", "out%d" % o2, out_v[:, :, tgs(tg)], oT[:, o2, :, :], reads=[("oT", o2)])
        for o2 in range(2):
            d = p.dsem["out%d" % o2]
            nc.sync.wait_ge(d[0], d[1])
    es.close()
    return nc


def prep_inputs(inp):
    f = lambda a: np.ascontiguousarray(np.asarray(a, dtype=np.float32))
    nm = f(inp["norm_mix_g"]).reshape(2, 8, 128).transpose(2, 0, 1).reshape(128, 16)
    nf = f(inp["norm_ffn_g"]).reshape(2, 8, 128).transpose(2, 0, 1).reshape(128, 16)
    fg = f(inp["final_norm_g"]).reshape(8, 128).T
    gains = f(np.concatenate([nm, nf, fg], axis=1))
    shared = {
        "gains": gains,
        "m_win": f(inp["mlstm_w_in"][0]),
        "m_bg": f(inp["mlstm_b_gate"]).reshape(1, 8),
        "m_ong": f(inp["mlstm_out_norm_g"]).reshape(1, 1024),
        "m_wout": f(inp["mlstm_w_out"][0]),
        "g_win": f(inp["gla_w_in"][0]),
        "g_wgu": f(inp["gla_w_gate_up"][0]),
        "g_bg": f(f(inp["gla_b_gate"]).reshape(4, 128).T),
        "g_ong": f(inp["gla_out_norm_g"]).reshape(1, 1024),
        "g_wout": f(inp["gla_w_out"][0]),
        "w1": f(inp["ffn_w1"]),
        "w2": f(inp["ffn_w2"]),
    }
    x = np.asarray(inp["x"], dtype=np.float32)
    in_maps = []
    for b in range(8):
        m = dict(shared)
        m["xT"] = np.ascontiguousarray(x[b].T)
        in_maps.append(m)
    return in_maps


def kernel(**inputs):
    in_maps = prep_inputs(inputs)
    nc = build()
    res = run_bass_kernel_spmd(nc, in_maps, core_ids=list(range(8)))
    out = np.stack([np.ascontiguousarray(res.results[b]["outT"].T) for b in range(8)], axis=0)
    return out.astype(np.float32)
```
